# Optimizing a Trainium2 kernel written in Bass

```python
import jax, jax.numpy as jnp
from jax import lax
import numpy as np

D_MODEL = 1024
BATCH = 16
SEQ = 2048
DEPTH = 1

CONV_WIDTH = D_MODEL // 2
CONV_K = 3
NSA_HEADS = 8
NSA_KV_GROUPS = 2
NSA_HEAD_DIM = 64
NSA_Q_WIDTH = NSA_HEADS * NSA_HEAD_DIM
NSA_KV_WIDTH = NSA_KV_GROUPS * NSA_HEAD_DIM
MIX_WIDTH = CONV_WIDTH + NSA_Q_WIDTH
N_BRANCH = 3
CMP_BLOCK = 32
CMP_STRIDE = 16
SEL_BLOCK = 64
SEL_TOPN = 16
WINDOW = 512
Q_BLOCK = 64
SPLIT_SIZES = (CONV_WIDTH, CONV_WIDTH, CONV_WIDTH, NSA_Q_WIDTH,
               N_BRANCH * 2 * NSA_KV_WIDTH, N_BRANCH * NSA_HEADS)
IN_COLS = sum(SPLIT_SIZES)

MEM_TOKENS = 256
X_HEADS = 4
X_HEAD_DIM = D_MODEL // X_HEADS

PEER_HEADS = 8
PEER_NKEYS = 128
PEER_EXPERTS = PEER_NKEYS * PEER_NKEYS
PEER_QDIM = 256
PEER_TOPK = 16
PEER_TOKEN_CHUNK = 128

EPS = 1e-6

kernel_name = "hymba_conv_nsa_peer_layer"


def rms_norm(x, w):
    xf = x.astype(jnp.float32)
    y = xf * lax.rsqrt(jnp.mean(xf * xf, axis=-1, keepdims=True) + EPS)
    return (y * w.astype(jnp.float32)).astype(x.dtype)


def masked_softmax(s, mask):
    s = jnp.where(mask, s.astype(jnp.float32), -jnp.inf)
    m = jnp.max(s, axis=-1, keepdims=True)
    m = jnp.where(jnp.isfinite(m), m, 0.0)
    e = jnp.exp(s - m)
    d = jnp.sum(e, axis=-1, keepdims=True)
    return e / jnp.where(d > 0, d, 1.0)


def short_conv_mixer(b_gate, c_gate, h, conv_w, conv_b):
    u = c_gate * h
    y = lax.conv_general_dilated(u, conv_w[:, None, :], window_strides=(1,),
                                 padding=((CONV_K - 1, 0),),
                                 dimension_numbers=('NWC', 'WIO', 'NWC'),
                                 feature_group_count=CONV_WIDTH)
    return b_gate * (y + conv_b)


def compress_blocks(t, pe, w1, w2):
    B, S, G, dh = t.shape
    n_cmp = (S - CMP_BLOCK) // CMP_STRIDE + 1
    idx = np.arange(n_cmp)[:, None] * CMP_STRIDE + np.arange(CMP_BLOCK)[None, :]
    blk = t[:, idx] + pe[None, None, :, None, :]
    blk = jnp.moveaxis(blk, 3, 2).reshape(B, n_cmp, G, CMP_BLOCK * dh)
    hid = jax.nn.gelu(blk @ w1, approximate=False)
    return hid @ w2


def nsa_mixer(q, kv, gates, cmp_pe, cmp_w1, cmp_w2, q_norm_w, k_norm_w):
    B, S = q.shape[0], q.shape[1]
    G, R, dh = NSA_KV_GROUPS, NSA_HEADS // NSA_KV_GROUPS, NSA_HEAD_DIM
    n_sel = S // SEL_BLOCK
    n_top = min(SEL_TOPN, n_sel)
    n_qb = S // Q_BLOCK
    q = rms_norm(q.reshape(B, S, G, R, dh), q_norm_w) * dh ** -0.5
    gates = gates.reshape(B, S, G, R, N_BRANCH)
    k_c, v_c, k_s, v_s, k_w, v_w = [t.reshape(B, S, G, dh)
                                    for t in jnp.split(kv, 2 * N_BRANCH, axis=-1)]
    k_cmp = rms_norm(compress_blocks(k_c, cmp_pe[0], cmp_w1[0], cmp_w2[0]), k_norm_w[0])
    v_cmp = compress_blocks(v_c, cmp_pe[1], cmp_w1[1], cmp_w2[1])
    n_cmp = k_cmp.shape[1]
    cmp_start = np.arange(n_cmp) * CMP_STRIDE
    cmp_last = cmp_start + CMP_BLOCK - 1
    sel_start = np.arange(n_sel) * SEL_BLOCK
    sel_map = jnp.asarray(((cmp_start[:, None] < sel_start[None, :] + SEL_BLOCK) &
                           (cmp_start[:, None] + CMP_BLOCK > sel_start[None, :])).astype(np.float32))
    to_blocks = lambda t: t.reshape(B, n_sel, SEL_BLOCK, G, dh).transpose(0, 3, 1, 2, 4)
    k_sb = to_blocks(rms_norm(k_s, k_norm_w[1]))
    v_sb = to_blocks(v_s)
    pad = ((0, 0), (WINDOW, 0), (0, 0), (0, 0))
    k_wp = jnp.pad(rms_norm(k_w, k_norm_w[2]), pad)
    v_wp = jnp.pad(v_w, pad)
    bi = jnp.arange(B)[:, None, None, None]
    gi = jnp.arange(G)[None, :, None, None]
    blk_ids = jnp.arange(n_sel)

    def query_block(i):
        t0 = i * Q_BLOCK
        tq = t0 + jnp.arange(Q_BLOCK)
        qb = lax.dynamic_slice_in_dim(q, t0, Q_BLOCK, axis=1)
        gb = lax.dynamic_slice_in_dim(gates, t0, Q_BLOCK, axis=1)
        s_c = jnp.einsum('bqgrd,bngd->bgrqn', qb, k_cmp)
        p_c = masked_softmax(s_c, cmp_last[None, :] <= tq[:, None])
        o_c = jnp.einsum('bgrqn,bngd->bqgrd', p_c.astype(v_cmp.dtype), v_cmp)
        imp = jnp.einsum('bgrqn,nj->bgqj', p_c, sel_map)
        cur = (tq // SEL_BLOCK)[:, None]
        future = blk_ids[None, :] > cur
        forced = (blk_ids[None, :] == 0) | (blk_ids[None, :] == cur) | (blk_ids[None, :] == cur - 1)
        imp = jnp.where(future, -jnp.inf, jnp.where(forced, jnp.inf, imp))
        _, sel = lax.top_k(imp, n_top)
        ks = k_sb[bi, gi, sel]
        vs = v_sb[bi, gi, sel]
        s_s = jnp.einsum('bqgrd,bgqkld->bgrqkl', qb, ks).reshape(B, G, R, Q_BLOCK, n_top * SEL_BLOCK)
        pos = sel[..., None] * SEL_BLOCK + jnp.arange(SEL_BLOCK)
        m_s = (pos <= tq[None, None, :, None, None]).reshape(B, G, 1, Q_BLOCK, n_top * SEL_BLOCK)
        p_s = masked_softmax(s_s, m_s).reshape(B, G, R, Q_BLOCK, n_top, SEL_BLOCK)
        o_s = jnp.einsum('bgrqkl,bgqkld->bqgrd', p_s.astype(vs.dtype), vs)
        kw = lax.dynamic_slice_in_dim(k_wp, t0, WINDOW + Q_BLOCK, axis=1)
        vw = lax.dynamic_slice_in_dim(v_wp, t0, WINDOW + Q_BLOCK, axis=1)
        sk = t0 - WINDOW + jnp.arange(WINDOW + Q_BLOCK)
        diff = tq[:, None] - sk[None, :]
        m_w = (sk[None, :] >= 0) & (diff >= 0) & (diff < WINDOW)
        s_w = jnp.einsum('bqgrd,bkgd->bgrqk', qb, kw)
        p_w = masked_softmax(s_w, m_w)
        o_w = jnp.einsum('bgrqk,bkgd->bqgrd', p_w.astype(vw.dtype), vw)
        return o_c * gb[..., 0:1] + o_s * gb[..., 1:2] + o_w * gb[..., 2:3]

    out = lax.map(query_block, jnp.arange(n_qb))
    return out.transpose(1, 0, 2, 3, 4, 5).reshape(B, S, NSA_Q_WIDTH)


def memory_cross_attention(h, mem, mem_norm_w, wq, wk, wv, wo, q_norm_w, k_norm_w):
    B, S, D = h.shape
    M = mem.shape[1]
    q = rms_norm((h @ wq).reshape(B, S, X_HEADS, X_HEAD_DIM), q_norm_w) * X_HEAD_DIM ** -0.5
    mn = rms_norm(mem, mem_norm_w)
    k = rms_norm((mn @ wk).reshape(B, M, X_HEADS, X_HEAD_DIM), k_norm_w)
    v = (mn @ wv).reshape(B, M, X_HEADS, X_HEAD_DIM)
    s = jnp.einsum('bshd,bmhd->bhsm', q, k).astype(jnp.float32)
    p = jax.nn.softmax(s, axis=-1).astype(v.dtype)
    o = jnp.einsum('bhsm,bmhd->bshd', p, v).reshape(B, S, D)
    return o @ wo


def peer_ffn(h, wq, keys, w_down, w_up):
    B, S, D = h.shape
    K = PEER_TOPK
    q = (h @ wq).reshape(B, S, PEER_HEADS, 2, PEER_QDIM // 2)
    sc = jnp.einsum('bshcd,hcnd->bshcn', q, keys).astype(jnp.float32)
    top_s, top_i = lax.top_k(sc, K)
    cand_s = (top_s[..., 0, :, None] + top_s[..., 1, None, :]).reshape(B, S, PEER_HEADS, K * K)
    cand_i = (top_i[..., 0, :, None] * PEER_NKEYS + top_i[..., 1, None, :]).reshape(B, S, PEER_HEADS, K * K)
    best_s, best_j = lax.top_k(cand_s, K)
    experts = jnp.take_along_axis(cand_i, best_j, axis=-1)
    gates = jax.nn.softmax(best_s, axis=-1).astype(h.dtype)
    n_chunks = (B * S) // PEER_TOKEN_CHUNK
    hc = h.reshape(n_chunks, PEER_TOKEN_CHUNK, D)
    ec = experts.reshape(n_chunks, PEER_TOKEN_CHUNK, PEER_HEADS, K)
    gc = gates.reshape(n_chunks, PEER_TOKEN_CHUNK, PEER_HEADS, K)

    def chunk(args):
        xt, e, g = args
        u = w_down[e]
        a = jax.nn.gelu(jnp.einsum('td,thkd->thk', xt, u), approximate=False)
        v = w_up[e]
        return jnp.einsum('thk,thkd->td', g * a, v)

    return lax.map(chunk, (hc, ec, gc)).reshape(B, S, D)


def setup_inputs(seed: int = 0) -> dict:
    key = jax.random.key(seed)
    ks = jax.random.split(key, 28)
    L, D = DEPTH, D_MODEL
    nrm = lambda k, shape, scale: jax.random.normal(k, shape, jnp.float32) * scale
    gain = lambda k, shape: 1.0 + 0.01 * jax.random.normal(k, shape, jnp.float32)
    return {
        "x": nrm(ks[0], (BATCH, SEQ, D), 1.0),
        "mem": nrm(ks[1], (BATCH, MEM_TOKENS, D), 1.0),
        "mix_norm_w": gain(ks[2], (L, D)),
        "w_in": nrm(ks[3], (L, D, IN_COLS), D ** -0.5),
        "conv_w": nrm(ks[4], (L, CONV_K, CONV_WIDTH), CONV_K ** -0.5),
        "conv_b": nrm(ks[5], (L, CONV_WIDTH), 0.01),
        "cmp_pe": nrm(ks[6], (L, 2, CMP_BLOCK, NSA_HEAD_DIM), 0.1),
        "cmp_w1": nrm(ks[7], (L, 2, CMP_BLOCK * NSA_HEAD_DIM, NSA_HEAD_DIM), (CMP_BLOCK * NSA_HEAD_DIM) ** -0.5),
        "cmp_w2": nrm(ks[8], (L, 2, NSA_HEAD_DIM, NSA_HEAD_DIM), NSA_HEAD_DIM ** -0.5),
        "q_norm_w": gain(ks[9], (L, NSA_HEAD_DIM)),
        "k_norm_w": gain(ks[10], (L, N_BRANCH, NSA_HEAD_DIM)),
        "w_out": nrm(ks[11], (L, MIX_WIDTH, D), MIX_WIDTH ** -0.5),
        "xattn_norm_w": gain(ks[12], (L, D)),
        "mem_norm_w": gain(ks[13], (L, D)),
        "xq": nrm(ks[14], (L, D, D), D ** -0.5),
        "xk": nrm(ks[15], (L, D, D), D ** -0.5),
        "xv": nrm(ks[16], (L, D, D), D ** -0.5),
        "xo": nrm(ks[17], (L, D, D), D ** -0.5),
        "xq_norm_w": gain(ks[18], (L, X_HEAD_DIM)),
        "xk_norm_w": gain(ks[19], (L, X_HEAD_DIM)),
        "ffn_norm_w": gain(ks[20], (L, D)),
        "peer_wq": nrm(ks[21], (L, D, PEER_HEADS * PEER_QDIM), D ** -0.5),
        "peer_keys": nrm(ks[22], (L, PEER_HEADS, 2, PEER_NKEYS, PEER_QDIM // 2), (PEER_QDIM // 2) ** -0.5),
        "peer_down": nrm(ks[23], (L, PEER_EXPERTS, D), D ** -0.5),
        "peer_up": nrm(ks[24], (L, PEER_EXPERTS, D), PEER_HEADS ** -0.5),
    }


def reference(x, mem, mix_norm_w, w_in, conv_w, conv_b, cmp_pe, cmp_w1, cmp_w2, q_norm_w, k_norm_w,
              w_out, xattn_norm_w, mem_norm_w, xq, xk, xv, xo, xq_norm_w, xk_norm_w,
              ffn_norm_w, peer_wq, peer_keys, peer_down, peer_up):
    offsets = np.cumsum(SPLIT_SIZES)[:-1].tolist()
    for l in range(DEPTH):
        hn = rms_norm(x, mix_norm_w[l])
        b_g, c_g, h_c, q, kv, g_lin = jnp.split(hn @ w_in[l], offsets, axis=-1)
        y_conv = short_conv_mixer(b_g, c_g, h_c, conv_w[l], conv_b[l])
        y_nsa = nsa_mixer(q, kv, jax.nn.sigmoid(g_lin), cmp_pe[l], cmp_w1[l], cmp_w2[l],
                          q_norm_w[l], k_norm_w[l])
        x = x + jnp.concatenate([y_conv, y_nsa], axis=-1) @ w_out[l]
        x = x + memory_cross_attention(rms_norm(x, xattn_norm_w[l]), mem, mem_norm_w[l],
                                       xq[l], xk[l], xv[l], xo[l], xq_norm_w[l], xk_norm_w[l])
        x = x + peer_ffn(rms_norm(x, ffn_norm_w[l]), peer_wq[l], peer_keys[l], peer_down[l], peer_up[l])
    return x
```

```python
import numpy as np
import concourse.bass as bass
import concourse.mybir as mybir
from concourse.bass_utils import run_bass_kernel_spmd
from contextlib import ExitStack

F32 = mybir.dt.float32
BF16 = mybir.dt.bfloat16
U32 = mybir.dt.uint32
ALU = mybir.AluOpType
ACTF = mybir.ActivationFunctionType
AX = mybir.AxisListType

NCORES = 8
NB = 2
S = 2048
DM = 1024
NT = S // 128
NEG = -240.0
EPS = 1e-6


class Unit:
    __slots__ = ("name", "last_w", "readers", "dma_readers", "sem", "dma_total")

    def __init__(self, name):
        self.name = name
        self.last_w = None
        self.readers = {}
        self.dma_readers = []
        self.sem = None
        self.dma_total = 0


class Op:
    __slots__ = ("eng", "fn", "deps", "is_dma", "unit", "done_val", "flag")

    def __init__(self, eng, fn, is_dma=False):
        self.eng = eng
        self.fn = fn
        self.deps = []
        self.is_dma = is_dma
        self.unit = None
        self.done_val = None
        self.flag = False


ENGS = ("pe", "act", "dve", "pool", "sp")


class Prog:
    def __init__(self, nc, stack):
        self.nc = nc
        self.stack = stack
        self.ops = []
        self.units = {}
        self.nsem = 0
        self.last = {}
        self.dma_since = []
        self.bar = {}
        self.muted = False
        self.deferq = None

    def unit(self, name):
        u = self.units.get(name)
        if u is None:
            u = Unit(name)
            self.units[name] = u
        return u

    def _dep(self, op, prev, kind):
        if prev is None or prev is op:
            return
        if prev.eng == "pe" and op.eng == "pe" and not prev.is_dma and not op.is_dma:
            return
        if (not prev.is_dma) and (not op.is_dma) and prev.eng == op.eng and kind == "RAR":
            return
        if prev.is_dma and op.is_dma and kind == "WAW":
            return
        op.deps.append(prev)

    def barrier(self):
        if self.muted:
            return
        deps = [o for o in self.last.values()] + list(self.dma_since)
        self.dma_since = []
        for e in ENGS:
            self.bar[e] = list(deps) + self.bar.get(e, [])

    def add(self, eng, fn, reads=(), writes=(), is_dma=False, sem_unit=None):
        if self.muted:
            return None
        if self.deferq is not None:
            self.deferq.append((eng, fn, reads, writes, is_dma, sem_unit))
            return None
        op = Op(eng, fn, is_dma)
        if self.bar.get(eng):
            for d in self.bar[eng]:
                if d.is_dma or d.eng != eng:
                    op.deps.append(d)
            self.bar[eng] = []
        reads = [self.unit(r) if isinstance(r, str) else r for r in reads]
        writes = [self.unit(w) if isinstance(w, str) else w for w in writes]
        for u in reads:
            self._dep(op, u.last_w, "RAW")
            if u.name.startswith("ps"):
                for r in u.readers.values():
                    self._dep(op, r, "RAR")
        for u in writes:
            self._dep(op, u.last_w, "WAW")
            for r in u.readers.values():
                self._dep(op, r, "WAR")
            for r in u.dma_readers:
                self._dep(op, r, "WAR")
        for u in reads:
            if is_dma:
                u.dma_readers.append(op)
            else:
                u.readers[eng] = op
        for u in writes:
            u.last_w = op
            u.readers = {}
            u.dma_readers = []
        if is_dma:
            su = sem_unit if sem_unit is not None else (writes[0] if writes else reads[0])
            if isinstance(su, str):
                su = self.unit(su)
            op.unit = su
            su.dma_total += 16
            op.done_val = su.dma_total
            self.dma_since.append(op)
        else:
            self.last[eng] = op
        self.ops.append(op)
        return op

    def replay(self, q, n):
        for _ in range(min(n, len(q))):
            self.add(*q.pop(0))

    def dma(self, q, out, in_, reads=(), writes=(), sem_unit=None, **kw):
        return self.add(q, lambda e: e.dma_start(out=out, in_=in_, **kw), reads, writes, True, sem_unit)

    def emit(self):
        nc = self.nc
        for op in self.ops:
            for d in op.deps:
                d.flag = True
        engs = {}
        for op in self.ops:
            engs.setdefault(op.eng, []).append(op)
        esem = {}
        for e in engs:
            esem[e] = self.stack.enter_context(nc.semaphore("es_" + e))
        for u in self.units.values():
            if u.dma_total > 0:
                u.sem = self.stack.enter_context(nc.semaphore("us_%d" % self.nsem))
                self.nsem += 1
        cnt = {e: 0 for e in engs}
        for op in self.ops:
            if not op.is_dma and op.flag:
                cnt[op.eng] += 1
                op.done_val = cnt[op.eng]
        block = self.stack.enter_context(nc.Block())
        units = self.units

        def run(engname, eobj):
            seen = {}
            for op in engs.get(engname, []):
                need = {}
                for d in op.deps:
                    s = d.unit.sem if d.is_dma else esem[d.eng]
                    v = d.done_val
                    key = id(s)
                    if v > need.get(key, (None, 0))[1]:
                        need[key] = (s, v)
                for key, (s, v) in need.items():
                    if seen.get(key, 0) >= v:
                        continue
                    seen[key] = v
                    eobj.wait_ge(s, v)
                ins = op.fn(eobj)
                if op.is_dma:
                    ins.then_inc(op.unit.sem, 16)
                elif op.flag:
                    ins.then_inc(esem[op.eng], 1)
            if engname == "sp":
                for u in units.values():
                    if u.name.startswith("OUT") and u.dma_total > 0:
                        eobj.wait_ge(u.sem, u.dma_total)

        if "pe" in engs:
            @block.tensor
            def _(e):
                run("pe", e)
        if "act" in engs:
            @block.scalar
            def _(e):
                run("act", e)
        if "dve" in engs:
            @block.vector
            def _(e):
                run("dve", e)
        if "pool" in engs:
            @block.gpsimd
            def _(e):
                run("pool", e)

        @block.sync
        def _(e):
            run("sp", e)


def host_consts():
    c = {}
    p = np.arange(128)
    c["ident"] = np.eye(128, dtype=np.float32)
    c["mcausal"] = np.where(p[:, None] <= p[None, :], 0.0, NEG).astype(np.float32)
    c["mband"] = np.where(p[:, None] > p[None, :], 0.0, NEG).astype(np.float32)
    n = np.arange(128)[:, None]
    tq = np.arange(S)[None, :]
    c["mcmp"] = np.where((16 * n + 31 <= tq) & (n < 127), 0.0, NEG).astype(np.float32)
    c["bones64"] = ((p[:, None] // 64) == (p[None, :] // 64)).astype(np.float32) / 64.0
    c["ones256"] = np.full((128, 128), 1.0 / 256.0, np.float32)
    E = np.zeros((32, NT * 128), np.float32)
    for j in range(NT):
        for q in range(128):
            E[2 * j + q // 64, j * 128 + q] = 1.0
    c["emat"] = E
    cs = np.arange(127) * 16
    ss = np.arange(32) * 64
    sm = ((cs[:, None] < ss[None, :] + 64) & (cs[:, None] + 32 > ss[None, :])).astype(np.float32)
    smp = np.zeros((128, 32), np.float32)
    smp[:127] = sm
    c["selmap"] = smp
    A = np.zeros((128, NT, 32), np.float32)
    Bm = np.zeros((128, NT, 32), np.float32)
    for i in range(NT):
        cur = (i * 128 + p) // 64
        j = np.arange(32)[None, :]
        future = j > cur[:, None]
        forced = (j == 0) | (j == cur[:, None]) | (j == cur[:, None] - 1)
        A[:, i, :] = np.where(future | forced, 0.0, 1.0)
        Bm[:, i, :] = np.where(future, -1e9, np.where(forced, 1e9, 0.0))
    c["tka"] = A
    c["tkb"] = Bm
    c["iota"] = np.tile(np.arange(128, dtype=np.float32)[None, :], (128, 1))
    return c


CONST_SHAPES = {
    "ident": [128, 128], "mcausal": [128, 128], "mband": [128, 128], "mcmp": [128, S],
    "bones64": [128, 128], "ones256": [128, 128], "emat": [32, NT * 128], "selmap": [128, 32],
    "tka": [128, NT, 32], "tkb": [128, NT, 32], "iota": [128, 128],
}

PARAM_SHAPES = {
    "x": [NB, S, DM], "mem": [NB, 256, DM],
    "w_in": [DM, 2840], "w_out": [DM, DM], "xq": [DM, DM], "xk": [DM, DM], "xv": [DM, DM], "xo": [DM, DM],
    "peer_wq": [DM, 2048], "keysT": [16, 128, 128], "wdT": [128, 128, 8, 128], "w_up": [16384, DM],
    "cmp_w1": [2, 2048, 64], "cmp_w2": [2, 64, 64], "peT": [2, 64, 32],
    "nw_mix": [128, 8], "nw_x": [128, 8], "nw_f": [128, 8], "nw_mem": [128, 8],
    "convw": [128, 4, 3], "convb": [128, 4], "qnw": [128, 1], "knw": [128, 3],
    "xqnw": [128, 2], "xknw": [128, 2],
}


def host_params(inp):
    f = lambda a: np.ascontiguousarray(a, dtype=np.float32)
    col = lambda v: f(v.reshape(8, 128).T)
    d = {}
    for k in ("w_in", "w_out", "xq", "xk", "xv", "xo", "peer_wq"):
        d[k] = f(inp[k][0])
    d["keysT"] = f(inp["peer_keys"][0].reshape(16, 128, 128).transpose(0, 2, 1))
    d["wdT"] = f(inp["peer_down"][0].reshape(128, 128, 8, 128).transpose(0, 3, 2, 1))
    d["w_up"] = f(inp["peer_up"][0])
    d["cmp_w1"] = f(inp["cmp_w1"][0])
    d["cmp_w2"] = f(inp["cmp_w2"][0])
    d["peT"] = f(inp["cmp_pe"][0].transpose(0, 2, 1))
    d["nw_mix"] = col(inp["mix_norm_w"][0])
    d["nw_x"] = col(inp["xattn_norm_w"][0])
    d["nw_f"] = col(inp["ffn_norm_w"][0])
    d["nw_mem"] = col(inp["mem_norm_w"][0])
    d["convw"] = f(inp["conv_w"][0].reshape(3, 4, 128).transpose(2, 1, 0))
    d["convb"] = f(inp["conv_b"][0].reshape(4, 128).T)
    d["qnw"] = f(np.tile(inp["q_norm_w"][0], 2).reshape(128, 1))
    d["knw"] = f(np.tile(inp["k_norm_w"][0], (1, 2)).T)
    d["xqnw"] = f(inp["xq_norm_w"][0].reshape(2, 128).T)
    d["xknw"] = f(inp["xk_norm_w"][0].reshape(2, 128).T)
    return d


class _Stop(Exception):
    pass


def build(nstage=3, dbg=False, nseq=NB, stop_at=None):
    nc = bass.Bass("TRN2", target_bir_lowering=False)
    dr = {}
    for k, shp in list(PARAM_SHAPES.items()) + list(CONST_SHAPES.items()):
        dr[k] = nc.dram_tensor(k, shp, F32, kind="ExternalInput").ap()
    out = nc.dram_tensor("out", [NB, S, DM], F32, kind="ExternalOutput").ap()
    wd_bf = nc.dram_tensor("wd_bf", [128, 128, 8, 128], BF16, kind="Internal").ap()
    wu_bf = nc.dram_tensor("wu_bf", [16384, DM], BF16, kind="Internal").ap()

    with ExitStack() as top:
        top.enter_context(nc.allow_low_precision("bf16 matmul operands, fp32 accumulation"))
        P = Prog(nc, top)
        cnt = [0]
        seqmode = [False]

        def chk(name):
            if stop_at == name:
                P.muted = True

        tcache = {}
        tocc = {}

        def sbt(st, name, shape, dt):
            k0 = (name, tuple(shape), str(dt))
            occ = tocc.get(k0, 0)
            tocc[k0] = occ + 1
            key = k0 + (occ,)
            if key in tcache:
                return tcache[key]
            cnt[0] += 1
            t = st.enter_context(nc.sbuf_tensor("%s_%d" % (name, cnt[0]), shape, dt))
            if seqmode[0]:
                tcache[key] = t
            return t

        psb = [top.enter_context(nc.psum_tensor("psb%d" % i, [128, 512], F32)) for i in range(7)]
        pst = top.enter_context(nc.psum_tensor("pst", [128, 1024], BF16))
        rot = {}

        def bank(group, banks):
            i = rot.get(group, 0)
            rot[group] = i + 1
            b = banks[i % len(banks)]
            return psb[b], "ps%d" % b

        def mm(o, l, r, start, stop, R, W):
            return P.add("pe", lambda e: e.matmul(o, l, r, start=start, stop=stop), R, W)

        def act(o, i, func, R, W, **kw):
            return P.add("act", lambda e: e.activation(out=o, in_=i, func=func, **kw), R, W)

        def dve(fn, R, W):
            return P.add("dve", fn, R, W)

        cs = top
        ident_bf = sbt(cs, "identb", [128, 128], BF16)
        ident_f = sbt(cs, "identf", [128, 128], F32)
        mcausal = sbt(cs, "mcausal", [128, 128], BF16)
        mband = sbt(cs, "mband", [128, 128], BF16)
        bones64 = sbt(cs, "bones64", [128, 128], BF16)
        ones256 = sbt(cs, "ones256", [128, 128], BF16)
        iota = sbt(cs, "iota", [128, 128], BF16)
        epst = sbt(cs, "epst", [128, 1], F32)
        for nm, t in (("ident", ident_bf), ("mcausal", mcausal), ("mband", mband),
                      ("bones64", bones64), ("ones256", ones256), ("iota", iota)):
            P.dma("pool", t[:], dr[nm], writes=["c_" + nm])
        P.dma("sp", ident_f[:], dr["ident"], writes=["c_identf"])
        dve(lambda e: e.memset(epst[:], EPS), [], ["c_eps"])
        CON = ["c_ident", "c_mcausal", "c_mband", "c_mcmp", "c_bones64", "c_ones256", "c_emat", "c_iota",
               "c_identf", "c_tka", "c_tkb", "c_eps"]
        nw = {}
        for nm in ("nw_mix", "nw_x", "nw_f", "nw_mem"):
            nw[nm] = sbt(cs, nm, [128, 8], F32)
            P.dma("sp", nw[nm][:], dr[nm], writes=["c_" + nm])
        convw = sbt(cs, "convw", [128, 4, 3], F32)
        convb = sbt(cs, "convb", [128, 4], F32)
        qnw = sbt(cs, "qnw", [128, 1], F32)
        knw = sbt(cs, "knw", [128, 3], F32)
        xqnw = sbt(cs, "xqnw", [128, 2], F32)
        xknw = sbt(cs, "xknw", [128, 2], F32)
        for nm, t in (("convw", convw), ("convb", convb), ("qnw", qnw), ("knw", knw), ("xqnw", xqnw), ("xknw", xknw)):
            P.dma("sp", t[:], dr[nm], writes=["c_" + nm])
        dve(lambda e: e.tensor_scalar(qnw[:], qnw[:], 0.125, None, ALU.mult), ["c_qnw"], ["c_qnw"])
        dve(lambda e: e.tensor_scalar(xqnw[:], xqnw[:], 1.0 / 16.0, None, ALU.mult), ["c_xqnw"], ["c_xqnw"])

        prepass = []

        def prepass_issue(n):
            for _ in range(min(n, len(prepass))):
                q_, o_, i_, u_ = prepass.pop(0)
                P.dma(q_, o_, i_, writes=[u_])

        if nstage >= 3:
            wd_src = dr["wdT"].rearrange("c p k e -> (c p) (k e)")
            wd_dst = wd_bf.rearrange("c p k e -> (c p) (k e)")
            for pc in range(16):
                rs = slice(pc * 1024, (pc + 1) * 1024)
                prepass.append(("pool", wd_dst[rs, :], wd_src[rs, :], "wdbf"))
                prepass.append(("pool", wu_bf[rs, :], dr["w_up"][rs, :], "wubf"))
        def rms_to_T(st_tmp, src_ap, src_units, nwt, nwu, dstT, dst_cols, dst_unit, tag):
            junk, ss, rt, rstd, xnb = st_tmp
            act(junk[:], src_ap, ACTF.Square, src_units, [tag + "junk", tag + "ss"], accum_out=ss[:])
            act(rt[:], ss[:], ACTF.Sqrt, [tag + "ss", "c_eps"], [tag + "rt"], scale=1.0 / DM, bias=epst[:])
            dve(lambda e: e.reciprocal(rstd[:], rt[:]), [tag + "rt"], [tag + "rstd"])
            dve(lambda e: e.tensor_scalar(xnb[:], src_ap, rstd[:], None, ALU.mult), src_units + [tag + "rstd"], [tag + "xnb"])
            for k in range(8):
                P.add("pe", lambda e, k=k: e.transpose(pst[:, k * 128:(k + 1) * 128], xnb[:, k * 128:(k + 1) * 128], ident_bf[:]),
                      [tag + "xnb", "c_ident"], ["pst"])
            dve(lambda e: e.tensor_tensor(dstT[:, :, dst_cols], pst[:].rearrange("p (k t) -> p k t", k=8),
                                          nwt[:].unsqueeze(2).to_broadcast([128, 8, 128]), ALU.mult),
                ["pst", nwu], [dst_unit])

        def norm_tmps(st):
            return (sbt(st, "junk", [128, DM], BF16), sbt(st, "ss", [128, 1], F32), sbt(st, "rt", [128, 1], F32),
                    sbt(st, "rstd", [128, 1], F32), sbt(st, "xnb", [128, DM], BF16))

        def load_w(dst_ap, src2d, c0, n, unit):
            P.dma("pool", dst_ap, src2d.rearrange("(k p) c -> p k c", p=128)[:, :, c0:c0 + n], writes=[unit])
            prepass_issue(1)

        def qknorm(st_tmp, ps_list, psu_list, ncol, onesm, onesu, wcols, wu, outs, outu, pbanks, tag):
            raw, sq, rt, rinv = st_tmp
            nt_ = len(ps_list)
            for t in range(nt_):
                act(raw[:, t, 0:ncol], ps_list[t], ACTF.Copy, [psu_list[t]], [tag + "raw%d" % t])
                act(sq[:, t, 0:ncol], ps_list[t], ACTF.Square, [psu_list[t]], [tag + "sq%d" % t])
            pb, pu = bank(tag + "n", pbanks)
            for t in range(nt_):
                mm(pb[:, 0:ncol], onesm[:], sq[:, t, 0:ncol], t == 0, t == nt_ - 1, [onesu, tag + "sq%d" % t], [pu])
            act(rt[:, 0:ncol], pb[:, 0:ncol], ACTF.Sqrt, [pu, "c_eps"], [tag + "rt"], bias=epst[:])
            dve(lambda e: e.reciprocal(rinv[:, 0:ncol], rt[:, 0:ncol]), [tag + "rt"], [tag + "rinv"])
            for t in range(nt_):
                dve(lambda e, t=t: e.scalar_tensor_tensor(outs[t], raw[:, t, 0:ncol], wcols[t], rinv[:, 0:ncol], ALU.mult, ALU.mult),
                    [tag + "raw%d" % t, tag + "rinv", wu], [outu[t]])

        def qk_tmps(st, nt_):
            return (sbt(st, "raw", [128, nt_, 512], F32), sbt(st, "sq", [128, nt_, 512], BF16),
                    sbt(st, "rt", [128, 512], F32), sbt(st, "rinv", [128, 512], F32))

        def tm_to_T(src_bf, src_unit, nchunk, dstT, dst_cols, dst_unit):
            for k in range(nchunk):
                P.add("pe", lambda e, k=k: e.transpose(pst[:, k * 128:(k + 1) * 128], src_bf[:, k * 128:(k + 1) * 128], ident_bf[:]),
                      [src_unit, "c_ident"], ["pst"])
            act(dstT[:, 0:nchunk, dst_cols], pst[:, 0:nchunk * 128].rearrange("p (k t) -> p k t", k=nchunk), ACTF.Copy,
                ["pst"], [dst_unit])

        def out_proj(yT, yu, wsb, wu_, xres, xru):
            for i in range(NT):
                for hf in range(2):
                    pb, pu = bank("op", [0, 1, 2, 3])
                    for k in range(8):
                        mm(pb[:, :], yT[:, k, i * 128:(i + 1) * 128], wsb[:, k, hf * 512:(hf + 1) * 512], k == 0, k == 7,
                           [yu, wu_], [pu])
                    dve(lambda e, i=i, hf=hf, pb=pb: e.tensor_tensor(xres[:, i, hf * 512:(hf + 1) * 512], pb[:, :],
                                                                     xres[:, i, hf * 512:(hf + 1) * 512], ALU.add),
                        [pu, xru(i)], [xru(i)])


        def peer_stage(b):
            prepass_issue(len(prepass))
            with ExitStack() as sp_:
                wq = sbt(sp_, "wq", [128, 8, 2048], BF16)
                load_w(wq[:, :, :], dr["peer_wq"], 0, 2048, "wq")
                keysT = sbt(sp_, "keysT", [128, 16, 128], BF16)
                P.dma("pool", keysT[:], dr["keysT"].rearrange("h d n -> d h n"), writes=["keysT"])
                Gg = sbt(sp_, "Gg", [128, 256, 128], BF16)
                Wd4 = [sbt(sp_, "Wd4", [128, 4, 8, 128], BF16) for _ in range(2)]
                Wu4 = [sbt(sp_, "Wu4", [128, 4, DM], BF16) for _ in range(2)]
                P0 = sbt(sp_, "P0", [128, 8, 128], BF16)
                P1 = sbt(sp_, "P1", [128, 8, 128], BF16)
                bufA = sbt(sp_, "bufA", [128, 2048], F32)
                bufB = sbt(sp_, "bufB", [128, 2048], F32)
                bufC = bufB
                ts = sbt(sp_, "ts", [128, 16, 16], F32)
                ti = sbt(sp_, "ti", [128, 16, 16], U32)
                tif = sbt(sp_, "tif", [128, 16, 16], F32)
                bs = sbt(sp_, "bs", [128, 8, 16], F32)
                bfi = sbt(sp_, "bfi", [128, 8, 16], U32)
                bff = sbt(sp_, "bff", [128, 8, 16], F32)
                ak = sbt(sp_, "ak", [128, 8, 16], F32)
                bk = sbt(sp_, "bk", [128, 8, 16], F32)
                gt = sbt(sp_, "gt", [128, 8, 16], F32)
                i0 = sbt(sp_, "i0", [128, 8, 16], F32)
                i1 = sbt(sp_, "i1", [128, 8, 16], F32)
                gsum = sbt(sp_, "gsum", [128, 8], F32)
                thr16 = sbt(sp_, "thr16", [128, 16], F32)
                dve(lambda e: e.tensor_scalar(thr16[:], iota[:, 0:16], 1.0, 16.0, ALU.add, ALU.mult), ["c_iota"], ["thr16"])
                i0T2 = [sbt(sp_, "i0T", [128, 256], F32) for _ in range(2)]
                i1T2 = [sbt(sp_, "i1T", [128, 256], F32) for _ in range(2)]
                gT2 = [sbt(sp_, "gT", [128, 256], F32) for _ in range(2)]
                xg2 = [sbt(sp_, "xg", [128, 2, DM], F32) for _ in range(2)]
                xnTg2 = [sbt(sp_, "xnTg", [128, 8, 256], BF16) for _ in range(2)]
                qg = sbt(sp_, "qg", [128, 16, 256], BF16)
                ntm = norm_tmps(sp_)
                Asb = [sbt(sp_, "Asb", [128, 256], BF16) for _ in range(2)]
                GA = [sbt(sp_, "GA", [128, 256], BF16) for _ in range(3)]
                ts4 = ts[:].rearrange("p (h c) k -> p h c k", c=2)
                tif4 = tif[:].rearrange("p (h c) k -> p h c k", c=2)
                B4 = [128, 8, 16, 16]
                wi = [0]
                def prep(gi):
                    sl = gi % 2
                    xg, xnTg, i0T, i1T, gT = xg2[sl], xnTg2[sl], i0T2[sl], i1T2[sl], gT2[sl]
                    U = lambda n: "%s_%d" % (n, sl)
                    for tt in range(2):
                        i = 2 * gi + tt
                        P.dma("sp", xg[:, tt, :], out[b, i * 128:(i + 1) * 128, :], reads=["OUT%d_%d" % (b, i)], writes=[U("xg%d" % tt)])
                        rms_to_T(ntm, xg[:, tt, :], [U("xg%d" % tt)], nw["nw_f"], "c_nw_f", xnTg, slice(tt * 128, (tt + 1) * 128), U("xnTg"), "nC")
                    for hc2 in range(8):
                        pb, pu = bank("pq6", [6])
                        for s_ in range(2):
                            hc = 2 * hc2 + s_
                            for k in range(8):
                                mm(pb[:, s_ * 256:(s_ + 1) * 256], wq[:, k, hc * 128:(hc + 1) * 128], xnTg[:, k, :], k == 0, k == 7, ["wq", U("xnTg")], [pu])
                        act(qg[:, 2 * hc2:2 * hc2 + 2, :], pb[:, :].rearrange("p (a t) -> p a t", a=2), ACTF.Copy, [pu], ["qg"])
                    for tt in range(2):
                        for j in range(4):
                            pb, pu = bank("pq6", [6])
                            for s_ in range(4):
                                hc = 4 * j + s_
                                mm(pb[:, s_ * 128:(s_ + 1) * 128], qg[:, hc, tt * 128:(tt + 1) * 128], keysT[:, hc, :], True, True, ["qg", "keysT"], [pu])
                            act(bufA[:, j * 512:(j + 1) * 512], pb[:, :], ACTF.Copy, [pu], ["bufA"])
                        HC = [(hc, slice(hc * 128, (hc + 1) * 128)) for hc in range(16)]
                        for hc, cs_ in HC:
                            dve(lambda e, hc=hc, cs_=cs_: e.max(out=ts[:, hc, 0:8], in_=bufA[:, cs_]), ["bufA"], ["tsa%d" % hc])
                        for hc, cs_ in HC:
                            dve(lambda e, hc=hc, cs_=cs_: e.max_index(out=ti[:, hc, 0:8], in_max=ts[:, hc, 0:8], in_values=bufA[:, cs_]), ["bufA", "tsa%d" % hc], ["tia%d" % hc])
                        for hc, cs_ in HC:
                            dve(lambda e, hc=hc, cs_=cs_: e.match_replace(out=bufB[:, cs_], in_to_replace=ts[:, hc, 0:8], in_values=bufA[:, cs_], imm_value=-1e30), ["bufA", "tsa%d" % hc], ["bufB%d" % hc])
                        for hc, cs_ in HC:
                            dve(lambda e, hc=hc, cs_=cs_: e.max(out=ts[:, hc, 8:16], in_=bufB[:, cs_]), ["bufB%d" % hc], ["tsb%d" % hc])
                        for hc, cs_ in HC:
                            dve(lambda e, hc=hc, cs_=cs_: e.max_index(out=ti[:, hc, 8:16], in_max=ts[:, hc, 8:16], in_values=bufB[:, cs_]), ["bufB%d" % hc, "tsb%d" % hc], ["tib%d" % hc])
                        dve(lambda e: e.tensor_copy(tif[:], ti[:]), ["tia%d" % x for x in range(16)] + ["tib%d" % x for x in range(16)], ["tif"])
                        dve(lambda e: e.tensor_tensor(bufA[:].rearrange("p (h a b) -> p h a b", h=8, a=16),
                                                      ts4[:, :, 0, :].unsqueeze(3).to_broadcast(B4), ts4[:, :, 1, :].unsqueeze(2).to_broadcast(B4), ALU.add),
                            ["tsa%d" % x for x in range(16)] + ["tsb%d" % x for x in range(16)], ["bufA"] + ["bufB%d" % x for x in range(16)])
                        HH = [(h, slice(h * 256, (h + 1) * 256)) for h in range(8)]
                        for h, cs_ in HH:
                            dve(lambda e, h=h, cs_=cs_: e.max(out=bs[:, h, 0:8], in_=bufA[:, cs_]), ["bufA"], ["bsa%d" % h])
                        for h, cs_ in HH:
                            dve(lambda e, h=h, cs_=cs_: e.max_index(out=bfi[:, h, 0:8], in_max=bs[:, h, 0:8], in_values=bufA[:, cs_]), ["bufA", "bsa%d" % h], ["bfa%d" % h])
                        for h, cs_ in HH:
                            dve(lambda e, h=h, cs_=cs_: e.match_replace(out=bufB[:, cs_], in_to_replace=bs[:, h, 0:8], in_values=bufA[:, cs_], imm_value=-1e30), ["bufA", "bsa%d" % h], ["bufB%d" % (2 * h), "bufB%d" % (2 * h + 1)])
                        for h, cs_ in HH:
                            dve(lambda e, h=h, cs_=cs_: e.max(out=bs[:, h, 8:16], in_=bufB[:, cs_]), ["bufB%d" % (2 * h), "bufB%d" % (2 * h + 1)], ["bsb%d" % h])
                        for h, cs_ in HH:
                            dve(lambda e, h=h, cs_=cs_: e.max_index(out=bfi[:, h, 8:16], in_max=bs[:, h, 8:16], in_values=bufB[:, cs_]), ["bufB%d" % (2 * h), "bsb%d" % h], ["bfb%d" % h])
                        dve(lambda e: e.tensor_tensor(gt[:], bs[:], bs[:, :, 0:1].to_broadcast([128, 8, 16]), ALU.subtract), ["bsa%d" % x for x in range(8)] + ["bsb%d" % x for x in range(8)], ["gt"])
                        act(gt[:], gt[:], ACTF.Exp, ["gt"], ["gt"])
                        dve(lambda e: e.tensor_reduce(gsum[:], gt[:], AX.X, ALU.add), ["gt"], ["gsum"])
                        dve(lambda e: e.reciprocal(gsum[:], gsum[:]), ["gsum"], ["gsum"])
                        dve(lambda e: e.tensor_tensor(gt[:], gt[:], gsum[:].unsqueeze(2).to_broadcast([128, 8, 16]), ALU.mult), ["gt", "gsum"], ["gt"])
                        dve(lambda e: e.tensor_copy(bff[:], bfi[:]), ["bfa%d" % x for x in range(8)] + ["bfb%d" % x for x in range(8)], ["bff"])
                        ohv = bufC[:].rearrange("p (h k a) -> p h k a", h=8, k=16)
                        dve(lambda e: e.tensor_tensor(ohv, bff[:].unsqueeze(3).to_broadcast(B4), thr16[:].unsqueeze(1).unsqueeze(1).to_broadcast(B4), ALU.is_ge),
                            ["bff", "thr16"], ["bufC"])
                        dve(lambda e: e.tensor_reduce(ak[:], ohv, AX.X, ALU.add), ["bufC"], ["ak"])
                        dve(lambda e: e.scalar_tensor_tensor(bk[:], ak[:], -16.0, bff[:], ALU.mult, ALU.add), ["ak", "bff"], ["bk"])
                        io16 = iota[:, 0:16].unsqueeze(1).unsqueeze(1).to_broadcast(B4)
                        for (sel, c_, dst, du) in ((ak, 0, i0, "i0"), (bk, 1, i1, "i1")):
                            dve(lambda e, sel=sel: e.tensor_tensor(ohv, io16, sel[:].unsqueeze(3).to_broadcast(B4), ALU.is_equal), ["ak", "bk", "c_iota"], ["bufC"])
                            dve(lambda e, c_=c_: e.tensor_tensor(ohv, ohv, tif4[:, :, c_, :].unsqueeze(2).to_broadcast(B4), ALU.mult), ["bufC", "tif"], ["bufC"])
                            dve(lambda e, dst=dst: e.tensor_reduce(dst[:], ohv, AX.X, ALU.add), ["bufC"], [du])
                        for (src, su_, dT, dTu) in ((i0, "i0", i0T, U("i0T")), (i1, "i1", i1T, U("i1T")), (gt, "gt", gT, U("gT"))):
                            tb, tu = bank("pq6", [6])
                            P.add("pe", lambda e, tb=tb, src=src: e.transpose(tb[:, 0:128], src[:].rearrange("p h k -> p (h k)"), ident_f[:, :]), [su_, "c_identf"], [tu])
                            act(dT[:, tt * 128:(tt + 1) * 128], tb[:, 0:128], ACTF.Copy, [tu], [dTu])
                P.deferq = []
                prep(0)
                q0 = P.deferq
                P.deferq = None
                P.replay(q0, len(q0))
                for gi in range(NT // 2):
                    sl = gi % 2
                    xg, xnTg, i0T, i1T, gT = xg2[sl], xnTg2[sl], i0T2[sl], i1T2[sl], gT2[sl]
                    U = lambda n, sl=sl: "%s_%d" % (n, sl)
                    qn = []
                    if gi + 1 < NT // 2:
                        P.deferq = []
                        prep(gi + 1)
                        qn = P.deferq
                        P.deferq = None
                    for t4 in range(64):
                        gb, gu = bank("pq", [4, 5, 6])
                        for s_ in range(4):
                            tok = t4 * 4 + s_
                            rr = tok % 8
                            dve(lambda e, tok=tok, rr=rr, i0T=i0T: e.tensor_scalar(P0[:, rr, :], iota[:, :], i0T[:, tok:tok + 1], None, ALU.is_equal),
                                [U("i0T"), "c_iota"], ["P0r%d" % rr])
                            dve(lambda e, tok=tok, rr=rr, i1T=i1T, gT=gT: e.tensor_scalar(P1[:, rr, :], iota[:, :], i1T[:, tok:tok + 1], gT[:, tok:tok + 1], ALU.is_equal, ALU.mult),
                                [U("i1T"), U("gT"), "c_iota"], ["P1r%d" % rr])
                            mm(gb[:, s_ * 128:(s_ + 1) * 128], P1[:, rr, :], P0[:, rr, :], True, True, ["P0r%d" % rr, "P1r%d" % rr], [gu])
                        act(Gg[:, t4 * 4:t4 * 4 + 4, :], gb[:, :].rearrange("p (t i) -> p t i", t=4), ACTF.Copy, [gu], ["Gg"])
                    pend = [None]

                    def up_mm(c, g_, wu, cc, wslot):
                        for tt in range(2):
                            for hf in range(2):
                                bi = tt * 2 + hf
                                mm(psb[bi][:, :], g_[:, tt * 128:(tt + 1) * 128], wu[:, cc, hf * 512:(hf + 1) * 512], c == 0, c == 127,
                                   ["GA%d" % (c % 3), "Wu4_%d" % wslot], ["ps%d" % bi])

                    for c4 in range(32):
                        wslot = wi[0] % 2
                        wi[0] += 1
                        wd, wu = Wd4[wslot], Wu4[wslot]
                        P.dma("sp", wd[:], wd_bf[c4 * 4:(c4 + 1) * 4].rearrange("c p k e -> p c k e"), reads=["wdbf"], writes=["Wd4_%d" % wslot])
                        P.dma("sp", wu[:], wu_bf[c4 * 512:(c4 + 1) * 512, :].rearrange("(c p) d -> p c d", p=128), reads=["wubf"], writes=["Wu4_%d" % wslot])
                        for cc in range(4):
                            c = c4 * 4 + cc
                            pb, pu = bank("pa", [4, 5])
                            for k in range(8):
                                mm(pb[:, 0:256], wd[:, cc, k, :], xnTg[:, k, :], k == 0, k == 7, ["Wd4_%d" % wslot, U("xnTg")], [pu])
                            a_ = Asb[c % 2]
                            g_ = GA[c % 3]
                            act(a_[:], pb[:, 0:256], ACTF.Gelu, [pu], ["Asb%d" % (c % 2)])
                            P.add("pool", lambda e, a_=a_, g_=g_, c=c: e.tensor_tensor(g_[:], a_[:], Gg[:, :, c], ALU.mult), ["Asb%d" % (c % 2), "Gg"], ["GA%d" % (c % 3)])
                            if pend[0] is not None:
                                up_mm(*pend[0])
                            pend[0] = (c, g_, wu, cc, wslot)
                            P.replay(qn, 5)
                    up_mm(*pend[0])
                    pend[0] = None
                    P.replay(qn, len(qn))
                    for tt in range(2):
                        i = 2 * gi + tt
                        for hf in range(2):
                            bi = tt * 2 + hf
                            dve(lambda e, tt=tt, hf=hf, bi=bi, xg=xg: e.tensor_tensor(xg[:, tt, hf * 512:(hf + 1) * 512], psb[bi][:, :], xg[:, tt, hf * 512:(hf + 1) * 512], ALU.add),
                                ["ps%d" % bi, U("xg%d" % tt)], [U("xg%d" % tt)])
                        P.dma("sp", out[b, i * 128:(i + 1) * 128, :], xg[:, tt, :], reads=[U("xg%d" % tt)], writes=["OUT%d_%d" % (b, i)])

        for b in range(nseq):
            P.barrier()
            seqmode[0] = True
            tocc.clear()
            with ExitStack() as sq_:
                xres = sbt(sq_, "xres", [128, NT, DM], F32)
                xru = lambda i: "xres%d" % i
                for i in range(NT):
                    P.dma("sp", xres[:, i, :], dr["x"][b, i * 128:(i + 1) * 128, :], writes=[xru(i)])

                try:
                  with ExitStack() as sa:
                      ycT = sbt(sa, "ycT", [128, 8, S], BF16)
                      qT = sbt(sa, "qT", [128, 4, S], BF16)
                      kcT = sbt(sa, "kcT", [128, 2, S], BF16)
                      ksT = sbt(sa, "ksT", [128, 2, S], BF16)
                      vtm = sbt(sa, "vtm", [128, NT, 2, 2, 65], BF16)
                      gates = sbt(sa, "gates", [128, NT, 24], F32)
                      with ExitStack() as sa2:
                          xnT = sbt(sa2, "xnT", [128, 8, S], BF16)
                          ntm = norm_tmps(sa2)
                          for i in range(NT):
                              rms_to_T(ntm, xres[:, i, :], [xru(i)], nw["nw_mix"], "c_nw_mix", xnT, slice(i * 128, (i + 1) * 128), "xnT", "nA")
                          chk("norm")
                          wst = [sbt(sa2, "wst", [128, 8, 384], BF16) for _ in range(2)]
                          sc_ = ExitStack()
                          bsb = sbt(sc_, "bsb", [128, S], BF16)
                          hsb = sbt(sc_, "hsb", [128, 512], F32)
                          ufull = sbt(sc_, "ufull", [128, S + 2], BF16)
                          yacc = sbt(sc_, "yacc", [128, S], F32)
                          dve(lambda e: e.memset(ufull[:, 0:2], 0.0), [], ["ufull"])
                          for ct in range(4):
                              w = wst[ct % 2]
                              wu_ = "wst%d" % (ct % 2)
                              for j in range(3):
                                  load_w(w[:, :, j * 128:(j + 1) * 128], dr["w_in"], j * 512 + ct * 128, 128, wu_)
                              for q in range(4):
                                  pbs = []
                                  for j in range(3):
                                      pb, pu = bank("cv", [0, 1, 2, 3, 4, 5])
                                      for k in range(8):
                                          mm(pb[:, :], w[:, k, j * 128:(j + 1) * 128], xnT[:, k, q * 512:(q + 1) * 512], k == 0, k == 7,
                                             [wu_, "xnT"], [pu])
                                      pbs.append((pb, pu))
                                  act(bsb[:, q * 512:(q + 1) * 512], pbs[0][0][:, :], ACTF.Copy, [pbs[0][1]], ["bsb"])
                                  act(hsb[:, :], pbs[2][0][:, :], ACTF.Copy, [pbs[2][1]], ["hsb"])
                                  dve(lambda e, q=q, pb=pbs[1][0]: e.tensor_tensor(ufull[:, 2 + q * 512:2 + (q + 1) * 512], pb[:, :], hsb[:, :], ALU.mult),
                                      [pbs[1][1], "hsb"], ["ufull"])
                              dve(lambda e, ct=ct: e.tensor_scalar(yacc[:, :], ufull[:, 0:S], convw[:, ct, 0:1], None, ALU.mult),
                                  ["ufull", "c_convw"], ["yacc"])
                              dve(lambda e, ct=ct: e.scalar_tensor_tensor(yacc[:, :], ufull[:, 1:S + 1], convw[:, ct, 1:2], yacc[:, :], ALU.mult, ALU.add),
                                  ["ufull", "c_convw", "yacc"], ["yacc"])
                              dve(lambda e, ct=ct: e.scalar_tensor_tensor(yacc[:, :], ufull[:, 2:S + 2], convw[:, ct, 2:3], yacc[:, :], ALU.mult, ALU.add),
                                  ["ufull", "c_convw", "yacc"], ["yacc"])
                              dve(lambda e, ct=ct: e.scalar_tensor_tensor(ycT[:, ct, :], yacc[:, :], convb[:, ct:ct + 1], bsb[:, :], ALU.add, ALU.mult),
                                  ["yacc", "c_convb", "bsb"], ["ycT%d" % ct])
                          sc_.close()
                          chk("conv")
                          P.barrier()
                          qkt = qk_tmps(sa2, 1)
                          for r in range(4):
                              w = wst[r % 2]
                              wu_ = "wst%d" % (r % 2)
                              load_w(w[:, :, 0:64], dr["w_in"], 1536 + r * 64, 64, wu_)
                              load_w(w[:, :, 64:128], dr["w_in"], 1536 + (4 + r) * 64, 64, wu_)
                              for q in range(4):
                                  pb, pu = bank("cv", [0, 1, 2, 3, 4, 5])
                                  for k in range(8):
                                      mm(pb[:, :], w[:, k, 0:128], xnT[:, k, q * 512:(q + 1) * 512], k == 0, k == 7, [wu_, "xnT"], [pu])
                                  qknorm(qkt, [pb[:, :]], [pu], 512, bones64, "c_bones64", [qnw[:, 0:1]], "c_qnw",
                                         [qT[:, r, q * 512:(q + 1) * 512]], ["qT%d" % r], [6], "qn")
                          chk("q")
                          for idx, j in enumerate((0, 1, 2, 4)):
                              w = wst[idx % 2]
                              wu_ = "wst%d" % (idx % 2)
                              load_w(w[:, :, 0:128], dr["w_in"], 2048 + j * 128, 128, wu_)
                              for q in range(4):
                                  pb, pu = bank("cv", [0, 1, 2, 3, 4, 5])
                                  for k in range(8):
                                      mm(pb[:, :], w[:, k, 0:128], xnT[:, k, q * 512:(q + 1) * 512], k == 0, k == 7, [wu_, "xnT"], [pu])
                                  if j in (0, 1):
                                      act(kcT[:, j, q * 512:(q + 1) * 512], pb[:, :], ACTF.Copy, [pu], ["kcT%d" % j])
                                  else:
                                      bi = 0 if j == 2 else 1
                                      qknorm(qkt, [pb[:, :]], [pu], 512, bones64, "c_bones64", [knw[:, 1 + bi:2 + bi]], "c_knw",
                                             [ksT[:, bi, q * 512:(q + 1) * 512]], ["ksT%d" % bi], [6], "qn")
                          chk("kv")
                          w = wst[0]
                          load_w(w[:, :, 0:128], dr["w_in"], 2048 + 3 * 128, 128, "wst0")
                          load_w(w[:, :, 128:256], dr["w_in"], 2048 + 5 * 128, 128, "wst0")
                          load_w(w[:, :, 256:280], dr["w_in"], 2816, 24, "wst0")
                          dve(lambda e: e.memset(vtm[:].rearrange("p a b c d -> p (a b c d)"), 1.0), [], ["vtm"])
                          for i in range(NT):
                              pb, pu = bank("cv", [0, 1, 2, 3, 4, 5])
                              for k in range(8):
                                  mm(pb[:, 0:280], xnT[:, k, i * 128:(i + 1) * 128], w[:, k, 0:280], k == 0, k == 7, ["wst0", "xnT"], [pu])
                              act(vtm[:, i, :, :, 0:64], pb[:, 0:256].rearrange("p (a g d) -> p a g d", a=2, g=2), ACTF.Copy, [pu], ["vtm"])
                              act(gates[:, i, :], pb[:, 256:280], ACTF.Sigmoid, [pu], ["gates"])
                      chk("vtm")
                      P.barrier()
                      with ExitStack() as sa3:
                          mcmp = sbt(sa3, "mcmp", [128, S], BF16)
                          emat = sbt(sa3, "emat", [32, NT * 128], BF16)
                          tka = sbt(sa3, "tka", [128, NT, 32], F32)
                          tkb = sbt(sa3, "tkb", [128, NT, 32], F32)
                          P.dma("pool", mcmp[:], dr["mcmp"], writes=["c_mcmp"])
                          P.dma("pool", emat[:], dr["emat"], writes=["c_emat"])
                          P.dma("sp", tka[:], dr["tka"], writes=["c_tka"])
                          P.dma("sp", tkb[:], dr["tkb"], writes=["c_tkb"])
                          w1sb = sbt(sa3, "w1sb", [128, 2, 32, 64], BF16)
                          w2k = sbt(sa3, "w2k", [64, 2, 128], BF16)
                          w2v = sbt(sa3, "w2v", [64, 64], BF16)
                          peTs = sbt(sa3, "peTs", [128, 2, 32], BF16)
                          for wh in range(2):
                              for hh in range(2):
                                  P.dma("pool", w1sb[hh * 64:(hh + 1) * 64, wh, :, :], dr["cmp_w1"][wh].rearrange("(l d) j -> d l j", d=64), writes=["w1sb"])
                                  P.dma("pool", peTs[hh * 64:(hh + 1) * 64, wh, :], dr["peT"][wh], writes=["peTs"])
                          P.add("pool", lambda e: e.memset(w2k[:, :, :], 0.0), [], ["w2k"])
                          for g in range(2):
                              P.dma("pool", w2k[:, g, g * 64:(g + 1) * 64], dr["cmp_w2"][0], reads=["w2k"], writes=["w2kk"])
                          P.dma("pool", w2v[:, :], dr["cmp_w2"][1], writes=["w2v"])
                          hid = sbt(sa3, "hid", [64, 2, 2, 128], BF16)
                          cbias = sbt(sa3, "cbias", [64, 2], F32)
                          kcmpT = sbt(sa3, "kcmpT", [128, 128], BF16)
                          vcx = sbt(sa3, "vcx", [128, 2, 97], BF16)
                          P.dma("pool", vcx[:, 0, 65:97], dr["selmap"], writes=["vcx"])
                          P.dma("pool", vcx[:, 1, 65:97], dr["selmap"], writes=["vcx"])
                          dve(lambda e: e.memset(vcx[:, :, 64:65], 1.0), ["vcx"], ["vcx"])
                          qkt = qk_tmps(sa3, 1)
                          for wh in range(2):
                              pb, pu = bank("cm", [0, 1])
                              for l in range(32):
                                  mm(pb[0:64, 0:1], w1sb[0:64, wh, l, :], peTs[0:64, wh, l:l + 1], l == 0, l == 31, ["w1sb", "peTs"], [pu])
                              act(cbias[:, wh:wh + 1], pb[0:64, 0:1], ACTF.Copy, [pu], ["cbias"])
                              for g in range(2):
                                  pb, pu = bank("cm", [0, 1])
                                  for l in range(32):
                                      mm(pb[0:64, 0:127], w1sb[g * 64:(g + 1) * 64, wh, l, :], kcT[g * 64:(g + 1) * 64, wh, l:l + 16 * 126 + 1:16],
                                         l == 0, l == 31, ["w1sb", "kcT%d" % wh], [pu])
                                  act(hid[:, wh, g, 0:127], pb[0:64, 0:127], ACTF.Gelu, [pu, "cbias"], ["hid"], bias=cbias[:, wh:wh + 1])
                          pb, pu = bank("cm", [0, 1])
                          for g in range(2):
                              mm(pb[:, 0:127], w2k[:, g, :], hid[:, 0, g, 0:127], g == 0, g == 1, ["w2kk", "hid"], [pu])
                          qknorm(qkt, [pb[:, 0:127]], [pu], 127, bones64, "c_bones64", [knw[:, 0:1]], "c_knw", [kcmpT[:, 0:127]], ["kcmpT"], [6], "qn")
                          for g in range(2):
                              pb, pu = bank("cm", [0, 1])
                              mm(pb[0:127, 0:64], hid[:, 1, g, 0:127], w2v[:, :], True, True, ["w2v", "hid"], [pu])
                              act(vcx[0:127, g, 0:64], pb[0:127, 0:64], ACTF.Copy, [pu], ["vcx"])
                          chk("cmp")
                          pexp = [sbt(sa3, "pexp", [128, 512], BF16) for _ in range(16)]
                          obuf = sbt(sa3, "obuf", [128, 8, 4, 65], F32)
                          den = sbt(sa3, "den", [128, 24], F32)
                          fac = sbt(sa3, "fac", [128, 24], F32)
                          yw = sbt(sa3, "yw", [128, 8, 3, 64], F32)
                          ynb = sbt(sa3, "ynb", [128, 512], BF16)
                          imp = sbt(sa3, "imp", [128, 2, 32], F32)
                          impw = sbt(sa3, "impw", [128, 4, 32], F32)
                          rdc = sbt(sa3, "rdc", [128, 4], F32)
                          m8a = sbt(sa3, "m8a", [128, 8], F32)
                          m8b = sbt(sa3, "m8b", [128, 8], F32)
                          imr = sbt(sa3, "imr", [128, 32], F32)
                          msel = sbt(sa3, "msel", [128, 32], F32)
                          lT = sbt(sa3, "lT", [32, 2, 128], BF16)
                          pxi = [0]
                          B4q = [128, 4, 128]

                          def branch_p1(i, g, kind, items):
                              gs = slice(g * 64, (g + 1) * 64)
                              qcols = slice(i * 128, (i + 1) * 128)
                              qu = ["qT0", "qT1", "qT2", "qT3"]
                              groups = []
                              for (j, mk) in items:
                                  sb_, su = bank("sc", [0, 1, 2, 3])
                                  if kind == 0:
                                      o3 = sb_[0:127, :].rearrange("p (r t) -> p r t", r=4)
                                      mm(o3, kcmpT[gs, 0:127], qT[gs, :, qcols], True, False, ["kcmpT"] + qu, [su])
                                      mm(o3, ident_bf[0:127, 0:127], mcmp[0:127, qcols].unsqueeze(1).to_broadcast([127, 4, 128]), False, True, ["c_ident", "c_mcmp"], [su])
                                  else:
                                      o3 = sb_[:, :].rearrange("p (r t) -> p r t", r=4)
                                      kt = ksT[gs, kind - 1, j * 128:(j + 1) * 128]
                                      mm(o3, kt, qT[gs, :, qcols], True, mk is None, ["ksT%d" % (kind - 1)] + qu, [su])
                                      if mk == "causal":
                                          mm(o3, ident_bf[:, :], mcausal[:, :].unsqueeze(1).to_broadcast(B4q), False, True, ["c_ident", "c_mcausal"], [su])
                                      elif mk == "band":
                                          mm(o3, ident_bf[:, :], mband[:, :].unsqueeze(1).to_broadcast(B4q), False, True, ["c_ident", "c_mband"], [su])
                                      elif mk == "sel":
                                          mm(o3, emat[:, j * 128:(j + 1) * 128], lT[:, g, :].unsqueeze(1).to_broadcast([32, 4, 128]), False, True, ["c_emat", "lT%d" % g], [su])
                                  px = pexp[pxi[0] % 16]
                                  pxu = "pexp%d" % (pxi[0] % 16)
                                  pxi[0] += 1
                                  nr = 127 if kind == 0 else 128
                                  act(px[0:nr, :], sb_[0:nr, :], ACTF.Exp, [su], [pxu])
                                  groups.append((j, px, pxu))
                              return (g, kind, groups)

                          def branch_p2(desc):
                              g, kind, groups = desc
                              ob, ou = bank("ob", [4, 5])
                              n = len(groups)
                              for r in range(4):
                                  for idx, (j, px, pxu) in enumerate(groups):
                                      if kind == 0:
                                          mm(ob[:, r * 97:(r + 1) * 97], px[0:127, r * 128:(r + 1) * 128], vcx[0:127, g, :], idx == 0, idx == n - 1, [pxu, "vcx"], [ou])
                                      else:
                                          mm(ob[:, r * 65:(r + 1) * 65], px[:, r * 128:(r + 1) * 128], vtm[:, j, kind - 1, g, :], idx == 0, idx == n - 1, [pxu, "vtm"], [ou])
                              return ob, ou

                          pendb = [None]

                          def flushb():
                              if pendb[0] is None:
                                  return
                              d_, post_ = pendb[0]
                              pendb[0] = None
                              ob_, ou_ = branch_p2(d_)
                              post_(ob_, ou_)

                          def submit(i, g, kind, items, post):
                              d_ = branch_p1(i, g, kind, items)
                              flushb()
                              pendb[0] = (d_, post)

                          def post_cmp(g):
                              def f(ob, ou):
                                  o3 = ob[:, 0:388].rearrange("p (r w) -> p r w", r=4)
                                  act(obuf[:, 4 * g:4 * g + 4, 0, :], o3[:, :, 0:65], ACTF.Copy, [ou], ["obuf"])
                                  dve(lambda e: e.tensor_scalar(rdc[:], o3[:, :, 64], 1e-30, None, ALU.max), [ou], ["rdc"])
                                  dve(lambda e: e.reciprocal(rdc[:], rdc[:]), ["rdc"], ["rdc"])
                                  dve(lambda e: e.tensor_tensor(impw[:], o3[:, :, 65:97], rdc[:].unsqueeze(2).to_broadcast([128, 4, 32]), ALU.mult), [ou, "rdc"], ["impw"])
                                  dve(lambda e: e.tensor_reduce(imp[:, g, :], impw[:].rearrange("p r j -> p j r"), AX.X, ALU.add), ["impw"], ["imp"])
                              return f

                          def post_o(g, slot):
                              def f(ob, ou):
                                  act(obuf[:, 4 * g:4 * g + 4, slot, :], ob[:, 0:260].rearrange("p (r w) -> p r w", r=4), ACTF.Copy, [ou], ["obuf"])
                              return f

                          for i in range(NT):
                              for g in range(2):
                                  submit(i, g, 0, [(0, "cmp")], post_cmp(g))
                                  flushb()
                                  dve(lambda e, g=g, i=i: e.tensor_tensor(imr[:], imp[:, g, :], tka[:, i, :], ALU.mult), ["imp", "c_tka"], ["imr"])
                                  dve(lambda e, i=i: e.tensor_tensor(imr[:], imr[:], tkb[:, i, :], ALU.add), ["imr", "c_tkb"], ["imr"])
                                  dve(lambda e: e.max(out=m8a[:], in_=imr[:]), ["imr"], ["m8a"])
                                  dve(lambda e: e.match_replace(out=msel[:], in_to_replace=m8a[:], in_values=imr[:], imm_value=-3e9), ["imr", "m8a"], ["msel"])
                                  dve(lambda e: e.max(out=m8b[:], in_=msel[:]), ["msel"], ["m8b"])
                                  dve(lambda e: e.tensor_scalar(msel[:], imr[:], m8b[:, 7:8], None, ALU.is_ge), ["imr", "m8b"], ["msel"])
                                  dve(lambda e: e.tensor_scalar(msel[:], msel[:], -1.0, -NEG, ALU.add, ALU.mult), ["msel"], ["msel"])
                                  tb, tu = bank("tp", [6])
                                  P.add("pe", lambda e, tb=tb: e.transpose(tb[0:32, 0:128], msel[:, :], ident_f[:, :]), ["msel", "c_identf"], [tu])
                                  act(lT[:, g, :], tb[0:32, 0:128], ACTF.Copy, [tu], ["lT%d" % g])
                                  witems = [(j, "causal" if j == i else ("band" if j == i - 4 else None)) for j in range(max(0, i - 4), i + 1)]
                                  submit(i, g, 2, witems, post_o(g, 2))
                                  sitems = [(j, "causal" if j == i else "sel") for j in range(i + 1)]
                                  submit(i, g, 1, sitems[0:8], post_o(g, 1))
                                  if len(sitems) > 8:
                                      submit(i, g, 1, sitems[8:], post_o(g, 3))
                              flushb()
                              if i >= 8:
                                  dve(lambda e: e.tensor_tensor(obuf[:, :, 1, :], obuf[:, :, 1, :], obuf[:, :, 3, :], ALU.add), ["obuf"], ["obuf"])
                              dve(lambda e: e.tensor_scalar(den[:].rearrange("p (h c) -> p h c", c=3), obuf[:, :, 0:3, 64], 1e-30, None, ALU.max), ["obuf"], ["den"])
                              dve(lambda e: e.reciprocal(den[:], den[:]), ["den"], ["den"])
                              dve(lambda e, i=i: e.tensor_tensor(fac[:], den[:], gates[:, i, :], ALU.mult), ["den", "gates"], ["fac"])
                              dve(lambda e: e.tensor_tensor(yw[:], obuf[:, :, 0:3, 0:64],
                                                            fac[:].rearrange("p (h c) -> p h c", c=3).unsqueeze(3).to_broadcast([128, 8, 3, 64]), ALU.mult),
                                  ["obuf", "fac"], ["yw"])
                              dve(lambda e: e.tensor_reduce(ynb[:].rearrange("p (h d) -> p h d", h=8), yw[:].rearrange("p h c d -> p h d c"), AX.X, ALU.add),
                                  ["yw"], ["ynb"])
                              for k in range(4):
                                  P.add("pe", lambda e, k=k: e.transpose(pst[:, k * 128:(k + 1) * 128], ynb[:, k * 128:(k + 1) * 128], ident_bf[:]),
                                        ["ynb", "c_ident"], ["pst"])
                              act(ycT[:, 4:8, i * 128:(i + 1) * 128], pst[:, 0:512].rearrange("p (k t) -> p k t", k=4), ACTF.Copy, ["pst"], ["ycTn"])
                      chk("nsa")
                      P.barrier()
                      with ExitStack() as sa4:
                          wo = sbt(sa4, "wo", [128, 8, DM], BF16)
                          load_w(wo[:, :, :], dr["w_out"], 0, DM, "wo")
                          out_proj(ycT, "ycTall", wo, "wo", xres, xru)
                except _Stop:
                    pass
                P.muted = False
                P.barrier()

                if nstage >= 2:
                    with ExitStack() as sbk:
                        xnT = sbt(sbk, "xnTb", [128, 8, S], BF16)
                        qTn = sbt(sbk, "qTn", [128, 8, S], BF16)
                        wA = sbt(sbk, "wA", [128, 8, DM], BF16)
                        wB = sbt(sbk, "wB", [128, 8, DM], BF16)
                        ntm = norm_tmps(sbk)
                        memsb = sbt(sbk, "memsb", [128, 1, DM], F32)
                        mnT = sbt(sbk, "mnT", [128, 8, 256], BF16)
                        kTn = sbt(sbk, "kTn", [128, 8, 256], BF16)
                        vx = sbt(sbk, "vx", [128, 2, 4, 257], BF16)
                        qkt2 = qk_tmps(sbk, 2)
                        pxb = [sbt(sbk, "pxb", [128, 2, 512], BF16) for _ in range(2)]
                        otm = sbt(sbk, "otm", [128, 4, DM], BF16)
                        rdx = sbt(sbk, "rdx", [128, 1], F32)
                        load_w(wA[:, :, :], dr["xk"], 0, DM, "wA")
                        load_w(wB[:, :, :], dr["xv"], 0, DM, "wB")
                        for mt in range(2):
                            P.dma("sp", memsb[:, 0, :], dr["mem"][b, mt * 128:(mt + 1) * 128, :], writes=["memsb"])
                            rms_to_T(ntm, memsb[:, 0, :], ["memsb"], nw["nw_mem"], "c_nw_mem", mnT, slice(mt * 128, (mt + 1) * 128), "mnT", "nB")
                        for i in range(NT):
                            rms_to_T(ntm, xres[:, i, :], [xru(i)], nw["nw_x"], "c_nw_x", xnT, slice(i * 128, (i + 1) * 128), "xnTb", "nB")
                        for h in range(4):
                            pl = []
                            for c in range(2):
                                pb, pu = bank("xb", [0, 1, 2, 3])
                                for k in range(8):
                                    mm(pb[:, 0:256], wA[:, k, (2 * h + c) * 128:(2 * h + c + 1) * 128], mnT[:, k, :], k == 0, k == 7, ["wA", "mnT"], [pu])
                                pl.append((pb, pu))
                            qknorm(qkt2, [pl[0][0][:, 0:256], pl[1][0][:, 0:256]], [pl[0][1], pl[1][1]], 256, ones256, "c_ones256",
                                   [xknw[:, 0:1], xknw[:, 1:2]], "c_xknw", [kTn[:, 2 * h, :], kTn[:, 2 * h + 1, :]], ["kTn", "kTn"], [6], "qx")
                        dve(lambda e: e.memset(vx[:].rearrange("p a b c -> p (a b c)"), 1.0), [], ["vx"])
                        for mt in range(2):
                            for hf in range(2):
                                pb, pu = bank("xb", [0, 1, 2, 3])
                                for k in range(8):
                                    mm(pb[:, :], mnT[:, k, mt * 128:(mt + 1) * 128], wB[:, k, hf * 512:(hf + 1) * 512], k == 0, k == 7, ["wB", "mnT"], [pu])
                                act(vx[:, mt, 2 * hf:2 * hf + 2, 0:256], pb[:, :].rearrange("p (a d) -> p a d", a=2), ACTF.Copy, [pu], ["vx"])
                        load_w(wA[:, :, :], dr["xq"], 0, DM, "wA")
                        load_w(wB[:, :, :], dr["xo"], 0, DM, "wB")
                        for h in range(4):
                            for q in range(4):
                                pl = []
                                for c in range(2):
                                    pb, pu = bank("xb", [0, 1, 2, 3])
                                    for k in range(8):
                                        mm(pb[:, :], wA[:, k, (2 * h + c) * 128:(2 * h + c + 1) * 128], xnT[:, k, q * 512:(q + 1) * 512], k == 0, k == 7, ["wA", "xnTb"], [pu])
                                    pl.append((pb, pu))
                                qknorm(qkt2, [pl[0][0][:, :], pl[1][0][:, :]], [pl[0][1], pl[1][1]], 512, ones256, "c_ones256",
                                       [xqnw[:, 0:1], xqnw[:, 1:2]], "c_xqnw",
                                       [qTn[:, 2 * h, q * 512:(q + 1) * 512], qTn[:, 2 * h + 1, q * 512:(q + 1) * 512]], ["qTn", "qTn"], [6], "qx")
                        pq = [0]
                        pendx = [None]

                        def xa_p1(q, h):
                            px = pxb[pq[0] % 2]
                            pxu = "pxb%d" % (pq[0] % 2)
                            pq[0] += 1
                            for mt in range(2):
                                sb_, su = bank("xs", [0, 1, 2, 3])
                                for c in range(2):
                                    mm(sb_[:, :], kTn[:, 2 * h + c, mt * 128:(mt + 1) * 128], qTn[:, 2 * h + c, q * 512:(q + 1) * 512], c == 0, c == 1, ["kTn", "qTn"], [su])
                                act(px[:, mt, :], sb_[:, :], ACTF.Exp, [su], [pxu])
                            return (h, px, pxu)

                        def xa_p2(d_):
                            h, px, pxu = d_
                            for t in range(4):
                                ob, ou = bank("xo", [4, 5])
                                for mt in range(2):
                                    mm(ob[:, 0:257], px[:, mt, t * 128:(t + 1) * 128], vx[:, mt, h, :], mt == 0, mt == 1, [pxu, "vx"], [ou])
                                dve(lambda e, ob=ob: e.reciprocal(rdx[:], ob[:, 256:257]), [ou], ["rdx"])
                                act(otm[:, t, h * 256:(h + 1) * 256], ob[:, 0:256], ACTF.Copy, [ou, "rdx"], ["otm"], scale=rdx[:])

                        for q in range(4):
                            for h in range(4):
                                d_ = xa_p1(q, h)
                                if pendx[0] is not None:
                                    xa_p2(pendx[0])
                                pendx[0] = d_
                            xa_p2(pendx[0])
                            pendx[0] = None
                            for t in range(4):
                                tm_to_T(otm[:, t, :], "otm", 8, xnT, slice((4 * q + t) * 128, (4 * q + t + 1) * 128), "xnTb")
                        out_proj(xnT, "xnTb", wB, "wB", xres, xru)
                    P.barrier()
                for i in range(NT):
                    P.dma("sp", out[b, i * 128:(i + 1) * 128, :], xres[:, i, :], reads=[xru(i)], writes=["OUT%d_%d" % (b, i)])
            if nstage >= 3:
                P.barrier()
                peer_stage(b)
        P.emit()
    return nc


_CACHE = {}


def kernel(**inputs):
    consts = host_consts()
    prm = host_params(inputs)
    if "nc" not in _CACHE:
        _CACHE["nc"] = build()
    nc = _CACHE["nc"]
    in_maps = []
    for c in range(NCORES):
        m = dict(prm)
        m.update(consts)
        m["x"] = np.ascontiguousarray(inputs["x"][c * NB:(c + 1) * NB], dtype=np.float32)
        m["mem"] = np.ascontiguousarray(inputs["mem"][c * NB:(c + 1) * NB], dtype=np.float32)
        in_maps.append(m)
    res = run_bass_kernel_spmd(nc, in_maps, core_ids=list(range(NCORES)))
    return np.concatenate([r["out"] for r in res.results], axis=0).astype(np.float32)
```

```python
import numpy as np
import concourse.bass as bass
import concourse.mybir as mybir
from concourse.bass_utils import run_bass_kernel_spmd
from contextlib import ExitStack

F32 = mybir.dt.float32
BF16 = mybir.dt.bfloat16
U32 = mybir.dt.uint32
ALU = mybir.AluOpType
ACTF = mybir.ActivationFunctionType
AX = mybir.AxisListType

NCORES = 8
NB = 2
S = 2048
DM = 1024
NT = S // 128
NEG = -240.0
EPS = 1e-6


class Unit:
    __slots__ = ("name", "last_w", "readers", "dma_readers", "sem", "dma_total")

    def __init__(self, name):
        self.name = name
        self.last_w = None
        self.readers = {}
        self.dma_readers = []
        self.sem = None
        self.dma_total = 0


class Op:
    __slots__ = ("eng", "fn", "deps", "is_dma", "unit", "done_val", "flag")

    def __init__(self, eng, fn, is_dma=False):
        self.eng = eng
        self.fn = fn
        self.deps = []
        self.is_dma = is_dma
        self.unit = None
        self.done_val = None
        self.flag = False


ENGS = ("pe", "act", "dve", "pool", "sp")


class Prog:
    def __init__(self, nc, stack):
        self.nc = nc
        self.stack = stack
        self.ops = []
        self.units = {}
        self.nsem = 0
        self.last = {}
        self.dma_since = []
        self.bar = {}
        self.muted = False
        self.deferq = None

    def unit(self, name):
        u = self.units.get(name)
        if u is None:
            u = Unit(name)
            self.units[name] = u
        return u

    def _dep(self, op, prev, kind):
        if prev is None or prev is op:
            return
        if prev.eng == "pe" and op.eng == "pe" and not prev.is_dma and not op.is_dma:
            return
        if (not prev.is_dma) and (not op.is_dma) and prev.eng == op.eng and kind == "RAR":
            return
        if prev.is_dma and op.is_dma and kind == "WAW":
            return
        op.deps.append(prev)

    def barrier(self):
        if self.muted:
            return
        deps = [o for o in self.last.values()] + list(self.dma_since)
        self.dma_since = []
        for e in ENGS:
            self.bar[e] = list(deps) + self.bar.get(e, [])

    def add(self, eng, fn, reads=(), writes=(), is_dma=False, sem_unit=None):
        if self.muted:
            return None
        if self.deferq is not None:
            self.deferq.append((eng, fn, reads, writes, is_dma, sem_unit))
            return None
        op = Op(eng, fn, is_dma)
        if self.bar.get(eng):
            for d in self.bar[eng]:
                if d.is_dma or d.eng != eng:
                    op.deps.append(d)
            self.bar[eng] = []
        reads = [self.unit(r) if isinstance(r, str) else r for r in reads]
        writes = [self.unit(w) if isinstance(w, str) else w for w in writes]
        for u in reads:
            self._dep(op, u.last_w, "RAW")
            if u.name.startswith("ps"):
                for r in u.readers.values():
                    self._dep(op, r, "RAR")
        for u in writes:
            self._dep(op, u.last_w, "WAW")
            for r in u.readers.values():
                self._dep(op, r, "WAR")
            for r in u.dma_readers:
                self._dep(op, r, "WAR")
        for u in reads:
            if is_dma:
                u.dma_readers.append(op)
            else:
                u.readers[eng] = op
        for u in writes:
            u.last_w = op
            u.readers = {}
            u.dma_readers = []
        if is_dma:
            su = sem_unit if sem_unit is not None else (writes[0] if writes else reads[0])
            if isinstance(su, str):
                su = self.unit(su)
            op.unit = su
            su.dma_total += 16
            op.done_val = su.dma_total
            self.dma_since.append(op)
        else:
            self.last[eng] = op
        self.ops.append(op)
        return op

    def replay(self, q, n):
        for _ in range(min(n, len(q))):
            self.add(*q.pop(0))

    def dma(self, q, out, in_, reads=(), writes=(), sem_unit=None, **kw):
        return self.add(q, lambda e: e.dma_start(out=out, in_=in_, **kw), reads, writes, True, sem_unit)

    def emit(self):
        nc = self.nc
        for op in self.ops:
            for d in op.deps:
                d.flag = True
        engs = {}
        for op in self.ops:
            engs.setdefault(op.eng, []).append(op)
        esem = {}
        for e in engs:
            esem[e] = self.stack.enter_context(nc.semaphore("es_" + e))
        for u in self.units.values():
            if u.dma_total > 0:
                u.sem = self.stack.enter_context(nc.semaphore("us_%d" % self.nsem))
                self.nsem += 1
        cnt = {e: 0 for e in engs}
        for op in self.ops:
            if not op.is_dma and op.flag:
                cnt[op.eng] += 1
                op.done_val = cnt[op.eng]
        block = self.stack.enter_context(nc.Block())
        units = self.units

        def run(engname, eobj):
            seen = {}
            for op in engs.get(engname, []):
                need = {}
                for d in op.deps:
                    s = d.unit.sem if d.is_dma else esem[d.eng]
                    v = d.done_val
                    key = id(s)
                    if v > need.get(key, (None, 0))[1]:
                        need[key] = (s, v)
                for key, (s, v) in need.items():
                    if seen.get(key, 0) >= v:
                        continue
                    seen[key] = v
                    eobj.wait_ge(s, v)
                ins = op.fn(eobj)
                if op.is_dma:
                    ins.then_inc(op.unit.sem, 16)
                elif op.flag:
                    ins.then_inc(esem[op.eng], 1)
            if engname == "sp":
                for u in units.values():
                    if u.name.startswith("OUT") and u.dma_total > 0:
                        eobj.wait_ge(u.sem, u.dma_total)

        if "pe" in engs:
            @block.tensor
            def _(e):
                run("pe", e)
        if "act" in engs:
            @block.scalar
            def _(e):
                run("act", e)
        if "dve" in engs:
            @block.vector
            def _(e):
                run("dve", e)
        if "pool" in engs:
            @block.gpsimd
            def _(e):
                run("pool", e)

        @block.sync
        def _(e):
            run("sp", e)


def host_consts():
    c = {}
    p = np.arange(128)
    c["ident"] = np.eye(128, dtype=np.float32)
    c["mcausal"] = np.where(p[:, None] <= p[None, :], 0.0, NEG).astype(np.float32)
    c["mband"] = np.where(p[:, None] > p[None, :], 0.0, NEG).astype(np.float32)
    n = np.arange(128)[:, None]
    tq = np.arange(S)[None, :]
    c["mcmp"] = np.where((16 * n + 31 <= tq) & (n < 127), 0.0, NEG).astype(np.float32)
    c["bones64"] = ((p[:, None] // 64) == (p[None, :] // 64)).astype(np.float32) / 64.0
    c["ones256"] = np.full((128, 128), 1.0 / 256.0, np.float32)
    E = np.zeros((32, NT * 128), np.float32)
    for j in range(NT):
        for q in range(128):
            E[2 * j + q // 64, j * 128 + q] = 1.0
    c["emat"] = E
    cs = np.arange(127) * 16
    ss = np.arange(32) * 64
    sm = ((cs[:, None] < ss[None, :] + 64) & (cs[:, None] + 32 > ss[None, :])).astype(np.float32)
    smp = np.zeros((128, 32), np.float32)
    smp[:127] = sm
    c["selmap"] = smp
    A = np.zeros((128, NT, 32), np.float32)
    Bm = np.zeros((128, NT, 32), np.float32)
    for i in range(NT):
        cur = (i * 128 + p) // 64
        j = np.arange(32)[None, :]
        future = j > cur[:, None]
        forced = (j == 0) | (j == cur[:, None]) | (j == cur[:, None] - 1)
        A[:, i, :] = np.where(future | forced, 0.0, 1.0)
        Bm[:, i, :] = np.where(future, -1e9, np.where(forced, 1e9, 0.0))
    c["tka"] = A
    c["tkb"] = Bm
    c["iota"] = np.tile(np.arange(128, dtype=np.float32)[None, :], (128, 1))
    return c


CONST_SHAPES = {
    "ident": [128, 128], "mcausal": [128, 128], "mband": [128, 128], "mcmp": [128, S],
    "bones64": [128, 128], "ones256": [128, 128], "emat": [32, NT * 128], "selmap": [128, 32],
    "tka": [128, NT, 32], "tkb": [128, NT, 32], "iota": [128, 128],
}

PARAM_SHAPES = {
    "x": [NB, S, DM], "mem": [NB, 256, DM],
    "w_in": [DM, 2840], "w_out": [DM, DM], "xq": [DM, DM], "xk": [DM, DM], "xv": [DM, DM], "xo": [DM, DM],
    "peer_wq": [DM, 2048], "keysT": [16, 128, 128], "wdT": [128, 128, 8, 128], "w_up": [16384, DM],
    "cmp_w1": [2, 2048, 64], "cmp_w2": [2, 64, 64], "peT": [2, 64, 32],
    "nw_mix": [128, 8], "nw_x": [128, 8], "nw_f": [128, 8], "nw_mem": [128, 8],
    "convw": [128, 4, 3], "convb": [128, 4], "qnw": [128, 1], "knw": [128, 3],
    "xqnw": [128, 2], "xknw": [128, 2],
}


def host_params(inp):
    f = lambda a: np.ascontiguousarray(a, dtype=np.float32)
    col = lambda v: f(v.reshape(8, 128).T)
    d = {}
    for k in ("w_in", "w_out", "xq", "xk", "xv", "xo", "peer_wq"):
        d[k] = f(inp[k][0])
    d["keysT"] = f(inp["peer_keys"][0].reshape(16, 128, 128).transpose(0, 2, 1))
    d["wdT"] = f(inp["peer_down"][0].reshape(128, 128, 8, 128).transpose(0, 3, 2, 1))
    d["w_up"] = f(inp["peer_up"][0])
    d["cmp_w1"] = f(inp["cmp_w1"][0])
    d["cmp_w2"] = f(inp["cmp_w2"][0])
    d["peT"] = f(inp["cmp_pe"][0].transpose(0, 2, 1))
    d["nw_mix"] = col(inp["mix_norm_w"][0])
    d["nw_x"] = col(inp["xattn_norm_w"][0])
    d["nw_f"] = col(inp["ffn_norm_w"][0])
    d["nw_mem"] = col(inp["mem_norm_w"][0])
    d["convw"] = f(inp["conv_w"][0].reshape(3, 4, 128).transpose(2, 1, 0))
    d["convb"] = f(inp["conv_b"][0].reshape(4, 128).T)
    d["qnw"] = f(np.tile(inp["q_norm_w"][0], 2).reshape(128, 1))
    d["knw"] = f(np.tile(inp["k_norm_w"][0], (1, 2)).T)
    d["xqnw"] = f(inp["xq_norm_w"][0].reshape(2, 128).T)
    d["xknw"] = f(inp["xk_norm_w"][0].reshape(2, 128).T)
    return d


class _Stop(Exception):
    pass


def build(nstage=3, dbg=False, nseq=NB, stop_at=None):
    nc = bass.Bass("TRN2", target_bir_lowering=False)
    dr = {}
    for k, shp in list(PARAM_SHAPES.items()) + list(CONST_SHAPES.items()):
        dr[k] = nc.dram_tensor(k, shp, F32, kind="ExternalInput").ap()
    out = nc.dram_tensor("out", [NB, S, DM], F32, kind="ExternalOutput").ap()
    wd_bf = nc.dram_tensor("wd_bf", [128, 128, 8, 128], BF16, kind="Internal").ap()
    wu_bf = nc.dram_tensor("wu_bf", [16384, DM], BF16, kind="Internal").ap()

    with ExitStack() as top:
        top.enter_context(nc.allow_low_precision("bf16 matmul operands, fp32 accumulation"))
        P = Prog(nc, top)
        cnt = [0]
        seqmode = [False]

        def chk(name):
            if stop_at == name:
                P.muted = True

        tcache = {}
        tocc = {}

        def sbt(st, name, shape, dt):
            k0 = (name, tuple(shape), str(dt))
            occ = tocc.get(k0, 0)
            tocc[k0] = occ + 1
            key = k0 + (occ,)
            if key in tcache:
                return tcache[key]
            cnt[0] += 1
            t = st.enter_context(nc.sbuf_tensor("%s_%d" % (name, cnt[0]), shape, dt))
            if seqmode[0]:
                tcache[key] = t
            return t

        psb = [top.enter_context(nc.psum_tensor("psb%d" % i, [128, 512], F32)) for i in range(7)]
        pst = top.enter_context(nc.psum_tensor("pst", [128, 1024], BF16))
        rot = {}

        def bank(group, banks):
            i = rot.get(group, 0)
            rot[group] = i + 1
            b = banks[i % len(banks)]
            return psb[b], "ps%d" % b

        def mm(o, l, r, start, stop, R, W):
            return P.add("pe", lambda e: e.matmul(o, l, r, start=start, stop=stop), R, W)

        def act(o, i, func, R, W, **kw):
            return P.add("act", lambda e: e.activation(out=o, in_=i, func=func, **kw), R, W)

        def dve(fn, R, W):
            return P.add("dve", fn, R, W)

        cs = top
        ident_bf = sbt(cs, "identb", [128, 128], BF16)
        ident_f = sbt(cs, "identf", [128, 128], F32)
        mcausal = sbt(cs, "mcausal", [128, 128], BF16)
        mband = sbt(cs, "mband", [128, 128], BF16)
        bones64 = sbt(cs, "bones64", [128, 128], BF16)
        ones256 = sbt(cs, "ones256", [128, 128], BF16)
        iota = sbt(cs, "iota", [128, 128], BF16)
        epst = sbt(cs, "epst", [128, 1], F32)
        for nm, t in (("ident", ident_bf), ("mcausal", mcausal), ("mband", mband),
                      ("bones64", bones64), ("ones256", ones256), ("iota", iota)):
            P.dma("pool", t[:], dr[nm], writes=["c_" + nm])
        P.dma("sp", ident_f[:], dr["ident"], writes=["c_identf"])
        dve(lambda e: e.memset(epst[:], EPS), [], ["c_eps"])
        CON = ["c_ident", "c_mcausal", "c_mband", "c_mcmp", "c_bones64", "c_ones256", "c_emat", "c_iota",
               "c_identf", "c_tka", "c_tkb", "c_eps"]
        nw = {}
        for nm in ("nw_mix", "nw_x", "nw_f", "nw_mem"):
            nw[nm] = sbt(cs, nm, [128, 8], F32)
            P.dma("sp", nw[nm][:], dr[nm], writes=["c_" + nm])
        convw = sbt(cs, "convw", [128, 4, 3], F32)
        convb = sbt(cs, "convb", [128, 4], F32)
        qnw = sbt(cs, "qnw", [128, 1], F32)
        knw = sbt(cs, "knw", [128, 3], F32)
        xqnw = sbt(cs, "xqnw", [128, 2], F32)
        xknw = sbt(cs, "xknw", [128, 2], F32)
        for nm, t in (("convw", convw), ("convb", convb), ("qnw", qnw), ("knw", knw), ("xqnw", xqnw), ("xknw", xknw)):
            P.dma("sp", t[:], dr[nm], writes=["c_" + nm])
        dve(lambda e: e.tensor_scalar(qnw[:], qnw[:], 0.125, None, ALU.mult), ["c_qnw"], ["c_qnw"])
        dve(lambda e: e.tensor_scalar(xqnw[:], xqnw[:], 1.0 / 16.0, None, ALU.mult), ["c_xqnw"], ["c_xqnw"])

        prepass = []

        def prepass_issue(n):
            for _ in range(min(n, len(prepass))):
                q_, o_, i_, u_ = prepass.pop(0)
                P.dma(q_, o_, i_, writes=[u_])

        if nstage >= 3:
            wd_src = dr["wdT"].rearrange("c p k e -> (c p) (k e)")
            wd_dst = wd_bf.rearrange("c p k e -> (c p) (k e)")
            for pc in range(16):
                rs = slice(pc * 1024, (pc + 1) * 1024)
                prepass.append(("pool", wd_dst[rs, :], wd_src[rs, :], "wdbf"))
                prepass.append(("pool", wu_bf[rs, :], dr["w_up"][rs, :], "wubf"))
        def rms_to_T(st_tmp, src_ap, src_units, nwt, nwu, dstT, dst_cols, dst_unit, tag):
            junk, ss, rt, rstd, xnb = st_tmp
            act(junk[:], src_ap, ACTF.Square, src_units, [tag + "junk", tag + "ss"], accum_out=ss[:])
            act(rt[:], ss[:], ACTF.Sqrt, [tag + "ss", "c_eps"], [tag + "rt"], scale=1.0 / DM, bias=epst[:])
            dve(lambda e: e.reciprocal(rstd[:], rt[:]), [tag + "rt"], [tag + "rstd"])
            dve(lambda e: e.tensor_scalar(xnb[:], src_ap, rstd[:], None, ALU.mult), src_units + [tag + "rstd"], [tag + "xnb"])
            for k in range(8):
                P.add("pe", lambda e, k=k: e.transpose(pst[:, k * 128:(k + 1) * 128], xnb[:, k * 128:(k + 1) * 128], ident_bf[:]),
                      [tag + "xnb", "c_ident"], ["pst"])
            dve(lambda e: e.tensor_tensor(dstT[:, :, dst_cols], pst[:].rearrange("p (k t) -> p k t", k=8),
                                          nwt[:].unsqueeze(2).to_broadcast([128, 8, 128]), ALU.mult),
                ["pst", nwu], [dst_unit])

        def norm_tmps(st):
            return (sbt(st, "junk", [128, DM], BF16), sbt(st, "ss", [128, 1], F32), sbt(st, "rt", [128, 1], F32),
                    sbt(st, "rstd", [128, 1], F32), sbt(st, "xnb", [128, DM], BF16))

        def load_w(dst_ap, src2d, c0, n, unit):
            P.dma("pool", dst_ap, src2d.rearrange("(k p) c -> p k c", p=128)[:, :, c0:c0 + n], writes=[unit])
            prepass_issue(1)

        def qknorm(st_tmp, ps_list, psu_list, ncol, onesm, onesu, wcols, wu, outs, outu, pbanks, tag):
            raw, sq, rt, rinv = st_tmp
            nt_ = len(ps_list)
            for t in range(nt_):
                act(raw[:, t, 0:ncol], ps_list[t], ACTF.Copy, [psu_list[t]], [tag + "raw%d" % t])
                act(sq[:, t, 0:ncol], ps_list[t], ACTF.Square, [psu_list[t]], [tag + "sq%d" % t])
            pb, pu = bank(tag + "n", pbanks)
            for t in range(nt_):
                mm(pb[:, 0:ncol], onesm[:], sq[:, t, 0:ncol], t == 0, t == nt_ - 1, [onesu, tag + "sq%d" % t], [pu])
            act(rt[:, 0:ncol], pb[:, 0:ncol], ACTF.Sqrt, [pu, "c_eps"], [tag + "rt"], bias=epst[:])
            dve(lambda e: e.reciprocal(rinv[:, 0:ncol], rt[:, 0:ncol]), [tag + "rt"], [tag + "rinv"])
            for t in range(nt_):
                dve(lambda e, t=t: e.scalar_tensor_tensor(outs[t], raw[:, t, 0:ncol], wcols[t], rinv[:, 0:ncol], ALU.mult, ALU.mult),
                    [tag + "raw%d" % t, tag + "rinv", wu], [outu[t]])

        def qk_tmps(st, nt_):
            return (sbt(st, "raw", [128, nt_, 512], F32), sbt(st, "sq", [128, nt_, 512], BF16),
                    sbt(st, "rt", [128, 512], F32), sbt(st, "rinv", [128, 512], F32))

        def tm_to_T(src_bf, src_unit, nchunk, dstT, dst_cols, dst_unit):
            for k in range(nchunk):
                P.add("pe", lambda e, k=k: e.transpose(pst[:, k * 128:(k + 1) * 128], src_bf[:, k * 128:(k + 1) * 128], ident_bf[:]),
                      [src_unit, "c_ident"], ["pst"])
            act(dstT[:, 0:nchunk, dst_cols], pst[:, 0:nchunk * 128].rearrange("p (k t) -> p k t", k=nchunk), ACTF.Copy,
                ["pst"], [dst_unit])

        def out_proj(yT, yu, wsb, wu_, xres, xru):
            for i in range(NT):
                for hf in range(2):
                    pb, pu = bank("op", [0, 1, 2, 3])
                    for k in range(8):
                        mm(pb[:, :], yT[:, k, i * 128:(i + 1) * 128], wsb[:, k, hf * 512:(hf + 1) * 512], k == 0, k == 7,
                           [yu, wu_], [pu])
                    dve(lambda e, i=i, hf=hf, pb=pb: e.tensor_tensor(xres[:, i, hf * 512:(hf + 1) * 512], pb[:, :],
                                                                     xres[:, i, hf * 512:(hf + 1) * 512], ALU.add),
                        [pu, xru(i)], [xru(i)])


        def peer_stage(b):
            prepass_issue(len(prepass))
            with ExitStack() as sp_:
                wq = sbt(sp_, "wq", [128, 8, 2048], BF16)
                load_w(wq[:, :, :], dr["peer_wq"], 0, 2048, "wq")
                keysT = sbt(sp_, "keysT", [128, 16, 128], BF16)
                P.dma("pool", keysT[:], dr["keysT"].rearrange("h d n -> d h n"), writes=["keysT"])
                Gg = sbt(sp_, "Gg", [128, 256, 128], BF16)
                Wd4 = [sbt(sp_, "Wd4", [128, 4, 8, 128], BF16) for _ in range(2)]
                Wu4 = [sbt(sp_, "Wu4", [128, 4, DM], BF16) for _ in range(2)]
                P0 = sbt(sp_, "P0", [128, 8, 128], BF16)
                P1 = sbt(sp_, "P1", [128, 8, 128], BF16)
                bufA = sbt(sp_, "bufA", [128, 2048], F32)
                bufB = sbt(sp_, "bufB", [128, 2048], F32)
                bufC = bufB
                ts = sbt(sp_, "ts", [128, 16, 16], F32)
                ti = sbt(sp_, "ti", [128, 16, 16], U32)
                tif = sbt(sp_, "tif", [128, 16, 16], F32)
                bs = sbt(sp_, "bs", [128, 8, 16], F32)
                bfi = sbt(sp_, "bfi", [128, 8, 16], U32)
                bff = sbt(sp_, "bff", [128, 8, 16], F32)
                ak = sbt(sp_, "ak", [128, 8, 16], F32)
                bk = sbt(sp_, "bk", [128, 8, 16], F32)
                gt = sbt(sp_, "gt", [128, 8, 16], F32)
                i0 = sbt(sp_, "i0", [128, 8, 16], F32)
                i1 = sbt(sp_, "i1", [128, 8, 16], F32)
                gsum = sbt(sp_, "gsum", [128, 8], F32)
                thr16 = sbt(sp_, "thr16", [128, 16], F32)
                dve(lambda e: e.tensor_scalar(thr16[:], iota[:, 0:16], 1.0, 16.0, ALU.add, ALU.mult), ["c_iota"], ["thr16"])
                i0T2 = [sbt(sp_, "i0T", [128, 256], F32) for _ in range(2)]
                i1T2 = [sbt(sp_, "i1T", [128, 256], F32) for _ in range(2)]
                gT2 = [sbt(sp_, "gT", [128, 256], F32) for _ in range(2)]
                xg2 = [sbt(sp_, "xg", [128, 2, DM], F32) for _ in range(2)]
                xnTg2 = [sbt(sp_, "xnTg", [128, 8, 256], BF16) for _ in range(2)]
                qg = sbt(sp_, "qg", [128, 16, 256], BF16)
                ntm = norm_tmps(sp_)
                Asb = [sbt(sp_, "Asb", [128, 256], BF16) for _ in range(2)]
                GA = [sbt(sp_, "GA", [128, 256], BF16) for _ in range(3)]
                ts4 = ts[:].rearrange("p (h c) k -> p h c k", c=2)
                tif4 = tif[:].rearrange("p (h c) k -> p h c k", c=2)
                B4 = [128, 8, 16, 16]
                wi = [0]
                def prep(gi):
                    sl = gi % 2
                    xg, xnTg, i0T, i1T, gT = xg2[sl], xnTg2[sl], i0T2[sl], i1T2[sl], gT2[sl]
                    U = lambda n: "%s_%d" % (n, sl)
                    for tt in range(2):
                        i = 2 * gi + tt
                        P.dma("sp", xg[:, tt, :], out[b, i * 128:(i + 1) * 128, :], reads=["OUT%d_%d" % (b, i)], writes=[U("xg%d" % tt)])
                        rms_to_T(ntm, xg[:, tt, :], [U("xg%d" % tt)], nw["nw_f"], "c_nw_f", xnTg, slice(tt * 128, (tt + 1) * 128), U("xnTg"), "nC")
                    for hc2 in range(8):
                        pb, pu = bank("pq6", [6])
                        for s_ in range(2):
                            hc = 2 * hc2 + s_
                            for k in range(8):
                                mm(pb[:, s_ * 256:(s_ + 1) * 256], wq[:, k, hc * 128:(hc + 1) * 128], xnTg[:, k, :], k == 0, k == 7, ["wq", U("xnTg")], [pu])
                        act(qg[:, 2 * hc2:2 * hc2 + 2, :], pb[:, :].rearrange("p (a t) -> p a t", a=2), ACTF.Copy, [pu], ["qg"])
                    for tt in range(2):
                        for j in range(4):
                            pb, pu = bank("pq6", [6])
                            for s_ in range(4):
                                hc = 4 * j + s_
                                mm(pb[:, s_ * 128:(s_ + 1) * 128], qg[:, hc, tt * 128:(tt + 1) * 128], keysT[:, hc, :], True, True, ["qg", "keysT"], [pu])
                            act(bufA[:, j * 512:(j + 1) * 512], pb[:, :], ACTF.Copy, [pu], ["bufA"])
                        HC = [(hc, slice(hc * 128, (hc + 1) * 128)) for hc in range(16)]
                        for hc, cs_ in HC:
                            dve(lambda e, hc=hc, cs_=cs_: e.max(out=ts[:, hc, 0:8], in_=bufA[:, cs_]), ["bufA"], ["tsa%d" % hc])
                        for hc, cs_ in HC:
                            dve(lambda e, hc=hc, cs_=cs_: e.max_index(out=ti[:, hc, 0:8], in_max=ts[:, hc, 0:8], in_values=bufA[:, cs_]), ["bufA", "tsa%d" % hc], ["tia%d" % hc])
                        for hc, cs_ in HC:
                            dve(lambda e, hc=hc, cs_=cs_: e.match_replace(out=bufB[:, cs_], in_to_replace=ts[:, hc, 0:8], in_values=bufA[:, cs_], imm_value=-1e30), ["bufA", "tsa%d" % hc], ["bufB%d" % hc])
                        for hc, cs_ in HC:
                            dve(lambda e, hc=hc, cs_=cs_: e.max(out=ts[:, hc, 8:16], in_=bufB[:, cs_]), ["bufB%d" % hc], ["tsb%d" % hc])
                        for hc, cs_ in HC:
                            dve(lambda e, hc=hc, cs_=cs_: e.max_index(out=ti[:, hc, 8:16], in_max=ts[:, hc, 8:16], in_values=bufB[:, cs_]), ["bufB%d" % hc, "tsb%d" % hc], ["tib%d" % hc])
                        dve(lambda e: e.tensor_copy(tif[:], ti[:]), ["tia%d" % x for x in range(16)] + ["tib%d" % x for x in range(16)], ["tif"])
                        dve(lambda e: e.tensor_tensor(bufA[:].rearrange("p (h a b) -> p h a b", h=8, a=16),
                                                      ts4[:, :, 0, :].unsqueeze(3).to_broadcast(B4), ts4[:, :, 1, :].unsqueeze(2).to_broadcast(B4), ALU.add),
                            ["tsa%d" % x for x in range(16)] + ["tsb%d" % x for x in range(16)], ["bufA"] + ["bufB%d" % x for x in range(16)])
                        HH = [(h, slice(h * 256, (h + 1) * 256)) for h in range(8)]
                        for h, cs_ in HH:
                            dve(lambda e, h=h, cs_=cs_: e.max(out=bs[:, h, 0:8], in_=bufA[:, cs_]), ["bufA"], ["bsa%d" % h])
                        for h, cs_ in HH:
                            dve(lambda e, h=h, cs_=cs_: e.max_index(out=bfi[:, h, 0:8], in_max=bs[:, h, 0:8], in_values=bufA[:, cs_]), ["bufA", "bsa%d" % h], ["bfa%d" % h])
                        for h, cs_ in HH:
                            dve(lambda e, h=h, cs_=cs_: e.match_replace(out=bufB[:, cs_], in_to_replace=bs[:, h, 0:8], in_values=bufA[:, cs_], imm_value=-1e30), ["bufA", "bsa%d" % h], ["bufB%d" % (2 * h), "bufB%d" % (2 * h + 1)])
                        for h, cs_ in HH:
                            dve(lambda e, h=h, cs_=cs_: e.max(out=bs[:, h, 8:16], in_=bufB[:, cs_]), ["bufB%d" % (2 * h), "bufB%d" % (2 * h + 1)], ["bsb%d" % h])
                        for h, cs_ in HH:
                            dve(lambda e, h=h, cs_=cs_: e.max_index(out=bfi[:, h, 8:16], in_max=bs[:, h, 8:16], in_values=bufB[:, cs_]), ["bufB%d" % (2 * h), "bsb%d" % h], ["bfb%d" % h])
                        dve(lambda e: e.tensor_tensor(gt[:], bs[:], bs[:, :, 0:1].to_broadcast([128, 8, 16]), ALU.subtract), ["bsa%d" % x for x in range(8)] + ["bsb%d" % x for x in range(8)], ["gt"])
                        act(gt[:], gt[:], ACTF.Exp, ["gt"], ["gt"])
                        dve(lambda e: e.tensor_reduce(gsum[:], gt[:], AX.X, ALU.add), ["gt"], ["gsum"])
                        dve(lambda e: e.reciprocal(gsum[:], gsum[:]), ["gsum"], ["gsum"])
                        dve(lambda e: e.tensor_tensor(gt[:], gt[:], gsum[:].unsqueeze(2).to_broadcast([128, 8, 16]), ALU.mult), ["gt", "gsum"], ["gt"])
                        dve(lambda e: e.tensor_copy(bff[:], bfi[:]), ["bfa%d" % x for x in range(8)] + ["bfb%d" % x for x in range(8)], ["bff"])
                        ohv = bufC[:].rearrange("p (h k a) -> p h k a", h=8, k=16)
                        dve(lambda e: e.tensor_tensor(ohv, bff[:].unsqueeze(3).to_broadcast(B4), thr16[:].unsqueeze(1).unsqueeze(1).to_broadcast(B4), ALU.is_ge),
                            ["bff", "thr16"], ["bufC"])
                        dve(lambda e: e.tensor_reduce(ak[:], ohv, AX.X, ALU.add), ["bufC"], ["ak"])
                        dve(lambda e: e.scalar_tensor_tensor(bk[:], ak[:], -16.0, bff[:], ALU.mult, ALU.add), ["ak", "bff"], ["bk"])
                        io16 = iota[:, 0:16].unsqueeze(1).unsqueeze(1).to_broadcast(B4)
                        for (sel, c_, dst, du) in ((ak, 0, i0, "i0"), (bk, 1, i1, "i1")):
                            dve(lambda e, sel=sel: e.tensor_tensor(ohv, io16, sel[:].unsqueeze(3).to_broadcast(B4), ALU.is_equal), ["ak", "bk", "c_iota"], ["bufC"])
                            dve(lambda e, c_=c_: e.tensor_tensor(ohv, ohv, tif4[:, :, c_, :].unsqueeze(2).to_broadcast(B4), ALU.mult), ["bufC", "tif"], ["bufC"])
                            dve(lambda e, dst=dst: e.tensor_reduce(dst[:], ohv, AX.X, ALU.add), ["bufC"], [du])
                        for (src, su_, dT, dTu) in ((i0, "i0", i0T, U("i0T")), (i1, "i1", i1T, U("i1T")), (gt, "gt", gT, U("gT"))):
                            tb, tu = bank("pq6", [6])
                            P.add("pe", lambda e, tb=tb, src=src: e.transpose(tb[:, 0:128], src[:].rearrange("p h k -> p (h k)"), ident_f[:, :]), [su_, "c_identf"], [tu])
                            act(dT[:, tt * 128:(tt + 1) * 128], tb[:, 0:128], ACTF.Copy, [tu], [dTu])
                P.deferq = []
                prep(0)
                q0 = P.deferq
                P.deferq = None
                P.replay(q0, len(q0))
                for gi in range(NT // 2):
                    sl = gi % 2
                    xg, xnTg, i0T, i1T, gT = xg2[sl], xnTg2[sl], i0T2[sl], i1T2[sl], gT2[sl]
                    U = lambda n, sl=sl: "%s_%d" % (n, sl)
                    qn = []
                    if gi + 1 < NT // 2:
                        P.deferq = []
                        prep(gi + 1)
                        qn = P.deferq
                        P.deferq = None
                    for t4 in range(64):
                        gb, gu = bank("pq", [4, 5, 6])
                        for s_ in range(4):
                            tok = t4 * 4 + s_
                            rr = tok % 8
                            dve(lambda e, tok=tok, rr=rr, i0T=i0T: e.tensor_scalar(P0[:, rr, :], iota[:, :], i0T[:, tok:tok + 1], None, ALU.is_equal),
                                [U("i0T"), "c_iota"], ["P0r%d" % rr])
                            dve(lambda e, tok=tok, rr=rr, i1T=i1T, gT=gT: e.tensor_scalar(P1[:, rr, :], iota[:, :], i1T[:, tok:tok + 1], gT[:, tok:tok + 1], ALU.is_equal, ALU.mult),
                                [U("i1T"), U("gT"), "c_iota"], ["P1r%d" % rr])
                            mm(gb[:, s_ * 128:(s_ + 1) * 128], P1[:, rr, :], P0[:, rr, :], True, True, ["P0r%d" % rr, "P1r%d" % rr], [gu])
                        act(Gg[:, t4 * 4:t4 * 4 + 4, :], gb[:, :].rearrange("p (t i) -> p t i", t=4), ACTF.Copy, [gu], ["Gg"])
                    pend = [None]

                    def up_mm(c, g_, wu, cc, wslot):
                        for tt in range(2):
                            for hf in range(2):
                                bi = tt * 2 + hf
                                mm(psb[bi][:, :], g_[:, tt * 128:(tt + 1) * 128], wu[:, cc, hf * 512:(hf + 1) * 512], c == 0, c == 127,
                                   ["GA%d" % (c % 3), "Wu4_%d" % wslot], ["ps%d" % bi])

                    for c4 in range(32):
                        wslot = wi[0] % 2
                        wi[0] += 1
                        wd, wu = Wd4[wslot], Wu4[wslot]
                        P.dma("sp", wd[:], wd_bf[c4 * 4:(c4 + 1) * 4].rearrange("c p k e -> p c k e"), reads=["wdbf"], writes=["Wd4_%d" % wslot])
                        P.dma("sp", wu[:], wu_bf[c4 * 512:(c4 + 1) * 512, :].rearrange("(c p) d -> p c d", p=128), reads=["wubf"], writes=["Wu4_%d" % wslot])
                        for cc in range(4):
                            c = c4 * 4 + cc
                            pb, pu = bank("pa", [4, 5])
                            for k in range(8):
                                mm(pb[:, 0:256], wd[:, cc, k, :], xnTg[:, k, :], k == 0, k == 7, ["Wd4_%d" % wslot, U("xnTg")], [pu])
                            a_ = Asb[c % 2]
                            g_ = GA[c % 3]
                            act(a_[:], pb[:, 0:256], ACTF.Gelu, [pu], ["Asb%d" % (c % 2)])
                            P.add("pool", lambda e, a_=a_, g_=g_, c=c: e.tensor_tensor(g_[:], a_[:], Gg[:, :, c], ALU.mult), ["Asb%d" % (c % 2), "Gg"], ["GA%d" % (c % 3)])
                            if pend[0] is not None:
                                up_mm(*pend[0])
                            pend[0] = (c, g_, wu, cc, wslot)
                            P.replay(qn, 5)
                    up_mm(*pend[0])
                    pend[0] = None
                    P.replay(qn, len(qn))
                    for tt in range(2):
                        i = 2 * gi + tt
                        for hf in range(2):
                            bi = tt * 2 + hf
                            dve(lambda e, tt=tt, hf=hf, bi=bi, xg=xg: e.tensor_tensor(xg[:, tt, hf * 512:(hf + 1) * 512], psb[bi][:, :], xg[:, tt, hf * 512:(hf + 1) * 512], ALU.add),
                                ["ps%d" % bi, U("xg%d" % tt)], [U("xg%d" % tt)])
                        P.dma("sp", out[b, i * 128:(i + 1) * 128, :], xg[:, tt, :], reads=[U("xg%d" % tt)], writes=["OUT%d_%d" % (b, i)])

        for b in range(nseq):
            P.barrier()
            seqmode[0] = True
            tocc.clear()
            with ExitStack() as sq_:
                xres = sbt(sq_, "xres", [128, NT, DM], F32)
                xru = lambda i: "xres%d" % i
                for i in range(NT):
                    P.dma("sp", xres[:, i, :], dr["x"][b, i * 128:(i + 1) * 128, :], writes=[xru(i)])

                try:
                  with ExitStack() as sa:
                      ycT = sbt(sa, "ycT", [128, 8, S], BF16)
                      qT = sbt(sa, "qT", [128, 4, S], BF16)
                      kcT = sbt(sa, "kcT", [128, 2, S], BF16)
                      ksT = sbt(sa, "ksT", [128, 2, S], BF16)
                      vtm = sbt(sa, "vtm", [128, NT, 2, 2, 65], BF16)
                      gates = sbt(sa, "gates", [128, NT, 24], F32)
                      with ExitStack() as sa2:
                          xnT = sbt(sa2, "xnT", [128, 8, S], BF16)
                          ntm = norm_tmps(sa2)
                          for i in range(NT):
                              rms_to_T(ntm, xres[:, i, :], [xru(i)], nw["nw_mix"], "c_nw_mix", xnT, slice(i * 128, (i + 1) * 128), "xnT", "nA")
                          chk("norm")
                          wst = [sbt(sa2, "wst", [128, 8, 384], BF16) for _ in range(2)]
                          sc_ = ExitStack()
                          bsb = sbt(sc_, "bsb", [128, S], BF16)
                          hsb = sbt(sc_, "hsb", [128, 512], F32)
                          ufull = sbt(sc_, "ufull", [128, S + 2], BF16)
                          yacc = sbt(sc_, "yacc", [128, S], F32)
                          dve(lambda e: e.memset(ufull[:, 0:2], 0.0), [], ["ufull"])
                          for ct in range(4):
                              w = wst[ct % 2]
                              wu_ = "wst%d" % (ct % 2)
                              for j in range(3):
                                  load_w(w[:, :, j * 128:(j + 1) * 128], dr["w_in"], j * 512 + ct * 128, 128, wu_)
                              for q in range(4):
                                  pbs = []
                                  for j in range(3):
                                      pb, pu = bank("cv", [0, 1, 2, 3, 4, 5])
                                      for k in range(8):
                                          mm(pb[:, :], w[:, k, j * 128:(j + 1) * 128], xnT[:, k, q * 512:(q + 1) * 512], k == 0, k == 7,
                                             [wu_, "xnT"], [pu])
                                      pbs.append((pb, pu))
                                  act(bsb[:, q * 512:(q + 1) * 512], pbs[0][0][:, :], ACTF.Copy, [pbs[0][1]], ["bsb"])
                                  act(hsb[:, :], pbs[2][0][:, :], ACTF.Copy, [pbs[2][1]], ["hsb"])
                                  dve(lambda e, q=q, pb=pbs[1][0]: e.tensor_tensor(ufull[:, 2 + q * 512:2 + (q + 1) * 512], pb[:, :], hsb[:, :], ALU.mult),
                                      [pbs[1][1], "hsb"], ["ufull"])
                              dve(lambda e, ct=ct: e.tensor_scalar(yacc[:, :], ufull[:, 0:S], convw[:, ct, 0:1], None, ALU.mult),
                                  ["ufull", "c_convw"], ["yacc"])
                              dve(lambda e, ct=ct: e.scalar_tensor_tensor(yacc[:, :], ufull[:, 1:S + 1], convw[:, ct, 1:2], yacc[:, :], ALU.mult, ALU.add),
                                  ["ufull", "c_convw", "yacc"], ["yacc"])
                              dve(lambda e, ct=ct: e.scalar_tensor_tensor(yacc[:, :], ufull[:, 2:S + 2], convw[:, ct, 2:3], yacc[:, :], ALU.mult, ALU.add),
                                  ["ufull", "c_convw", "yacc"], ["yacc"])
                              dve(lambda e, ct=ct: e.scalar_tensor_tensor(ycT[:, ct, :], yacc[:, :], convb[:, ct:ct + 1], bsb[:, :], ALU.add, ALU.mult),
                                  ["yacc", "c_convb", "bsb"], ["ycT%d" % ct])
                          sc_.close()
                          chk("conv")
                          P.barrier()
                          qkt = qk_tmps(sa2, 1)
                          for r in range(4):
                              w = wst[r % 2]
                              wu_ = "wst%d" % (r % 2)
                              load_w(w[:, :, 0:64], dr["w_in"], 1536 + r * 64, 64, wu_)
                              load_w(w[:, :, 64:128], dr["w_in"], 1536 + (4 + r) * 64, 64, wu_)
                              for q in range(4):
                                  pb, pu = bank("cv", [0, 1, 2, 3, 4, 5])
                                  for k in range(8):
                                      mm(pb[:, :], w[:, k, 0:128], xnT[:, k, q * 512:(q + 1) * 512], k == 0, k == 7, [wu_, "xnT"], [pu])
                                  qknorm(qkt, [pb[:, :]], [pu], 512, bones64, "c_bones64", [qnw[:, 0:1]], "c_qnw",
                                         [qT[:, r, q * 512:(q + 1) * 512]], ["qT%d" % r], [6], "qn")
                          chk("q")
                          for idx, j in enumerate((0, 1, 2, 4)):
                              w = wst[idx % 2]
                              wu_ = "wst%d" % (idx % 2)
                              load_w(w[:, :, 0:128], dr["w_in"], 2048 + j * 128, 128, wu_)
                              for q in range(4):
                                  pb, pu = bank("cv", [0, 1, 2, 3, 4, 5])
                                  for k in range(8):
                                      mm(pb[:, :], w[:, k, 0:128], xnT[:, k, q * 512:(q + 1) * 512], k == 0, k == 7, [wu_, "xnT"], [pu])
                                  if j in (0, 1):
                                      act(kcT[:, j, q * 512:(q + 1) * 512], pb[:, :], ACTF.Copy, [pu], ["kcT%d" % j])
                                  else:
                                      bi = 0 if j == 2 else 1
                                      qknorm(qkt, [pb[:, :]], [pu], 512, bones64, "c_bones64", [knw[:, 1 + bi:2 + bi]], "c_knw",
                                             [ksT[:, bi, q * 512:(q + 1) * 512]], ["ksT%d" % bi], [6], "qn")
                          chk("kv")
                          w = wst[0]
                          load_w(w[:, :, 0:128], dr["w_in"], 2048 + 3 * 128, 128, "wst0")
                          load_w(w[:, :, 128:256], dr["w_in"], 2048 + 5 * 128, 128, "wst0")
                          load_w(w[:, :, 256:280], dr["w_in"], 2816, 24, "wst0")
                          dve(lambda e: e.memset(vtm[:].rearrange("p a b c d -> p (a b c d)"), 1.0), [], ["vtm"])
                          for i in range(NT):
                              pb, pu = bank("cv", [0, 1, 2, 3, 4, 5])
                              for k in range(8):
                                  mm(pb[:, 0:280], xnT[:, k, i * 128:(i + 1) * 128], w[:, k, 0:280], k == 0, k == 7, ["wst0", "xnT"], [pu])
                              act(vtm[:, i, :, :, 0:64], pb[:, 0:256].rearrange("p (a g d) -> p a g d", a=2, g=2), ACTF.Copy, [pu], ["vtm"])
                              act(gates[:, i, :], pb[:, 256:280], ACTF.Sigmoid, [pu], ["gates"])
                      chk("vtm")
                      P.barrier()
                      with ExitStack() as sa3:
                          mcmp = sbt(sa3, "mcmp", [128, S], BF16)
                          emat = sbt(sa3, "emat", [32, NT * 128], BF16)
                          tka = sbt(sa3, "tka", [128, NT, 32], F32)
                          tkb = sbt(sa3, "tkb", [128, NT, 32], F32)
                          P.dma("pool", mcmp[:], dr["mcmp"], writes=["c_mcmp"])
                          P.dma("pool", emat[:], dr["emat"], writes=["c_emat"])
                          P.dma("sp", tka[:], dr["tka"], writes=["c_tka"])
                          P.dma("sp", tkb[:], dr["tkb"], writes=["c_tkb"])
                          w1sb = sbt(sa3, "w1sb", [128, 2, 32, 64], BF16)
                          w2k = sbt(sa3, "w2k", [64, 2, 128], BF16)
                          w2v = sbt(sa3, "w2v", [64, 64], BF16)
                          peTs = sbt(sa3, "peTs", [128, 2, 32], BF16)
                          for wh in range(2):
                              for hh in range(2):
                                  P.dma("pool", w1sb[hh * 64:(hh + 1) * 64, wh, :, :], dr["cmp_w1"][wh].rearrange("(l d) j -> d l j", d=64), writes=["w1sb"])
                                  P.dma("pool", peTs[hh * 64:(hh + 1) * 64, wh, :], dr["peT"][wh], writes=["peTs"])
                          P.add("pool", lambda e: e.memset(w2k[:, :, :], 0.0), [], ["w2k"])
                          for g in range(2):
                              P.dma("pool", w2k[:, g, g * 64:(g + 1) * 64], dr["cmp_w2"][0], reads=["w2k"], writes=["w2kk"])
                          P.dma("pool", w2v[:, :], dr["cmp_w2"][1], writes=["w2v"])
                          hid = sbt(sa3, "hid", [64, 2, 2, 128], BF16)
                          cbias = sbt(sa3, "cbias", [64, 2], F32)
                          kcmpT = sbt(sa3, "kcmpT", [128, 128], BF16)
                          vcx = sbt(sa3, "vcx", [128, 2, 97], BF16)
                          P.dma("pool", vcx[:, 0, 65:97], dr["selmap"], writes=["vcx"])
                          P.dma("pool", vcx[:, 1, 65:97], dr["selmap"], writes=["vcx"])
                          dve(lambda e: e.memset(vcx[:, :, 64:65], 1.0), ["vcx"], ["vcx"])
                          qkt = qk_tmps(sa3, 1)
                          for wh in range(2):
                              pb, pu = bank("cm", [0, 1])
                              for l in range(32):
                                  mm(pb[0:64, 0:1], w1sb[0:64, wh, l, :], peTs[0:64, wh, l:l + 1], l == 0, l == 31, ["w1sb", "peTs"], [pu])
                              act(cbias[:, wh:wh + 1], pb[0:64, 0:1], ACTF.Copy, [pu], ["cbias"])
                              for g in range(2):
                                  pb, pu = bank("cm", [0, 1])
                                  for l in range(32):
                                      mm(pb[0:64, 0:127], w1sb[g * 64:(g + 1) * 64, wh, l, :], kcT[g * 64:(g + 1) * 64, wh, l:l + 16 * 126 + 1:16],
                                         l == 0, l == 31, ["w1sb", "kcT%d" % wh], [pu])
                                  act(hid[:, wh, g, 0:127], pb[0:64, 0:127], ACTF.Gelu, [pu, "cbias"], ["hid"], bias=cbias[:, wh:wh + 1])
                          pb, pu = bank("cm", [0, 1])
                          for g in range(2):
                              mm(pb[:, 0:127], w2k[:, g, :], hid[:, 0, g, 0:127], g == 0, g == 1, ["w2kk", "hid"], [pu])
                          qknorm(qkt, [pb[:, 0:127]], [pu], 127, bones64, "c_bones64", [knw[:, 0:1]], "c_knw", [kcmpT[:, 0:127]], ["kcmpT"], [6], "qn")
                          for g in range(2):
                              pb, pu = bank("cm", [0, 1])
                              mm(pb[0:127, 0:64], hid[:, 1, g, 0:127], w2v[:, :], True, True, ["w2v", "hid"], [pu])
                              act(vcx[0:127, g, 0:64], pb[0:127, 0:64], ACTF.Copy, [pu], ["vcx"])
                          chk("cmp")
                          pexp = [sbt(sa3, "pexp", [128, 512], BF16) for _ in range(16)]
                          obuf = sbt(sa3, "obuf", [128, 8, 4, 65], F32)
                          den = sbt(sa3, "den", [128, 24], F32)
                          fac = sbt(sa3, "fac", [128, 24], F32)
                          yw = sbt(sa3, "yw", [128, 8, 3, 64], F32)
                          ynb = sbt(sa3, "ynb", [128, 512], BF16)
                          imp = sbt(sa3, "imp", [128, 2, 32], F32)
                          impw = sbt(sa3, "impw", [128, 4, 32], F32)
                          rdc = sbt(sa3, "rdc", [128, 4], F32)
                          m8a = sbt(sa3, "m8a", [128, 2, 8], F32)
                          m8b = sbt(sa3, "m8b", [128, 2, 8], F32)
                          imr = sbt(sa3, "imr", [128, 2, 32], F32)
                          msel = sbt(sa3, "msel", [128, 2, 32], F32)
                          lT = sbt(sa3, "lT", [32, 2, 128], BF16)
                          pxi = [0]
                          B4q = [128, 4, 128]

                          def branch_p1(i, g, kind, items):
                              gs = slice(g * 64, (g + 1) * 64)
                              qcols = slice(i * 128, (i + 1) * 128)
                              qu = ["qT0", "qT1", "qT2", "qT3"]
                              groups = []
                              for (j, mk) in items:
                                  sb_, su = bank("sc", [0, 1, 2, 3])
                                  if kind == 0:
                                      o3 = sb_[0:127, :].rearrange("p (r t) -> p r t", r=4)
                                      mm(o3, kcmpT[gs, 0:127], qT[gs, :, qcols], True, False, ["kcmpT"] + qu, [su])
                                      mm(o3, ident_bf[0:127, 0:127], mcmp[0:127, qcols].unsqueeze(1).to_broadcast([127, 4, 128]), False, True, ["c_ident", "c_mcmp"], [su])
                                  else:
                                      o3 = sb_[:, :].rearrange("p (r t) -> p r t", r=4)
                                      kt = ksT[gs, kind - 1, j * 128:(j + 1) * 128]
                                      mm(o3, kt, qT[gs, :, qcols], True, mk is None, ["ksT%d" % (kind - 1)] + qu, [su])
                                      if mk == "causal":
                                          mm(o3, ident_bf[:, :], mcausal[:, :].unsqueeze(1).to_broadcast(B4q), False, True, ["c_ident", "c_mcausal"], [su])
                                      elif mk == "band":
                                          mm(o3, ident_bf[:, :], mband[:, :].unsqueeze(1).to_broadcast(B4q), False, True, ["c_ident", "c_mband"], [su])
                                      elif mk == "sel":
                                          mm(o3, emat[:, j * 128:(j + 1) * 128], lT[:, g, :].unsqueeze(1).to_broadcast([32, 4, 128]), False, True, ["c_emat", "lT%d" % g], [su])
                                  px = pexp[pxi[0] % 16]
                                  pxu = "pexp%d" % (pxi[0] % 16)
                                  pxi[0] += 1
                                  nr = 127 if kind == 0 else 128
                                  act(px[0:nr, :], sb_[0:nr, :], ACTF.Exp, [su], [pxu])
                                  groups.append((j, px, pxu))
                              return (g, kind, groups)

                          def branch_p2(desc):
                              g, kind, groups = desc
                              ob, ou = bank("ob", [4, 5])
                              n = len(groups)
                              for r in range(4):
                                  for idx, (j, px, pxu) in enumerate(groups):
                                      if kind == 0:
                                          mm(ob[:, r * 97:(r + 1) * 97], px[0:127, r * 128:(r + 1) * 128], vcx[0:127, g, :], idx == 0, idx == n - 1, [pxu, "vcx"], [ou])
                                      else:
                                          mm(ob[:, r * 65:(r + 1) * 65], px[:, r * 128:(r + 1) * 128], vtm[:, j, kind - 1, g, :], idx == 0, idx == n - 1, [pxu, "vtm"], [ou])
                              return ob, ou

                          pendb = [None]

                          def flushb():
                              if pendb[0] is None:
                                  return
                              d_, post_ = pendb[0]
                              pendb[0] = None
                              ob_, ou_ = branch_p2(d_)
                              post_(ob_, ou_)

                          def submit(i, g, kind, items, post):
                              d_ = branch_p1(i, g, kind, items)
                              flushb()
                              pendb[0] = (d_, post)

                          def post_cmp(g):
                              def f(ob, ou):
                                  o3 = ob[:, 0:388].rearrange("p (r w) -> p r w", r=4)
                                  act(obuf[:, 4 * g:4 * g + 4, 0, :], o3[:, :, 0:65], ACTF.Copy, [ou], ["obuf"])
                                  dve(lambda e: e.tensor_scalar(rdc[:], o3[:, :, 64], 1e-30, None, ALU.max), [ou], ["rdc"])
                                  dve(lambda e: e.reciprocal(rdc[:], rdc[:]), ["rdc"], ["rdc"])
                                  dve(lambda e: e.tensor_tensor(impw[:], o3[:, :, 65:97], rdc[:].unsqueeze(2).to_broadcast([128, 4, 32]), ALU.mult), [ou, "rdc"], ["impw"])
                                  dve(lambda e: e.tensor_reduce(imp[:, g, :], impw[:].rearrange("p r j -> p j r"), AX.X, ALU.add), ["impw"], ["imp"])
                              return f

                          def post_o(g, slot):
                              def f(ob, ou):
                                  act(obuf[:, 4 * g:4 * g + 4, slot, :], ob[:, 0:260].rearrange("p (r w) -> p r w", r=4), ACTF.Copy, [ou], ["obuf"])
                              return f

                          def topk_mask(i, g):
                              dve(lambda e, g=g, i=i: e.tensor_tensor(imr[:, g, :], imp[:, g, :], tka[:, i, :], ALU.mult), ["imp", "c_tka"], ["imr%d" % g])
                              dve(lambda e, g=g, i=i: e.tensor_tensor(imr[:, g, :], imr[:, g, :], tkb[:, i, :], ALU.add), ["imr%d" % g, "c_tkb"], ["imr%d" % g])
                              dve(lambda e, g=g: e.max(out=m8a[:, g, :], in_=imr[:, g, :]), ["imr%d" % g], ["m8a%d" % g])
                              dve(lambda e, g=g: e.match_replace(out=msel[:, g, :], in_to_replace=m8a[:, g, :], in_values=imr[:, g, :], imm_value=-3e9),
                                  ["imr%d" % g, "m8a%d" % g], ["msel%d" % g])
                              dve(lambda e, g=g: e.max(out=m8b[:, g, :], in_=msel[:, g, :]), ["msel%d" % g], ["m8b%d" % g])
                              dve(lambda e, g=g: e.tensor_scalar(msel[:, g, :], imr[:, g, :], m8b[:, g, 7:8], None, ALU.is_ge), ["imr%d" % g, "m8b%d" % g], ["msel%d" % g])
                              dve(lambda e, g=g: e.tensor_scalar(msel[:, g, :], msel[:, g, :], -1.0, -NEG, ALU.add, ALU.mult), ["msel%d" % g], ["msel%d" % g])

                          def topk_T(g):
                              tb, tu = bank("tp", [6])
                              P.add("pe", lambda e, tb=tb, g=g: e.transpose(tb[0:32, 0:128], msel[:, g, :], ident_f[:, :]), ["msel%d" % g, "c_identf"], [tu])
                              act(lT[:, g, :], tb[0:32, 0:128], ACTF.Copy, [tu], ["lT%d" % g])

                          for i in range(NT):
                              for g in range(2):
                                  submit(i, g, 0, [(0, "cmp")], post_cmp(g))
                              flushb()
                              for g in range(2):
                                  topk_mask(i, g)
                              witems = [(j, "causal" if j == i else ("band" if j == i - 4 else None)) for j in range(max(0, i - 4), i + 1)]
                              for g in range(2):
                                  submit(i, g, 2, witems, post_o(g, 2))
                              for g in range(2):
                                  topk_T(g)
                              sitems = [(j, "causal" if j == i else "sel") for j in range(i + 1)]
                              for g in range(2):
                                  submit(i, g, 1, sitems[0:8], post_o(g, 1))
                                  if len(sitems) > 8:
                                      submit(i, g, 1, sitems[8:], post_o(g, 3))
                              flushb()
                              if i >= 8:
                                  dve(lambda e: e.tensor_tensor(obuf[:, :, 1, :], obuf[:, :, 1, :], obuf[:, :, 3, :], ALU.add), ["obuf"], ["obuf"])
                              dve(lambda e: e.tensor_scalar(den[:].rearrange("p (h c) -> p h c", c=3), obuf[:, :, 0:3, 64], 1e-30, None, ALU.max), ["obuf"], ["den"])
                              dve(lambda e: e.reciprocal(den[:], den[:]), ["den"], ["den"])
                              dve(lambda e, i=i: e.tensor_tensor(fac[:], den[:], gates[:, i, :], ALU.mult), ["den", "gates"], ["fac"])
                              dve(lambda e: e.tensor_tensor(yw[:], obuf[:, :, 0:3, 0:64],
                                                            fac[:].rearrange("p (h c) -> p h c", c=3).unsqueeze(3).to_broadcast([128, 8, 3, 64]), ALU.mult),
                                  ["obuf", "fac"], ["yw"])
                              dve(lambda e: e.tensor_reduce(ynb[:].rearrange("p (h d) -> p h d", h=8), yw[:].rearrange("p h c d -> p h d c"), AX.X, ALU.add),
                                  ["yw"], ["ynb"])
                              for k in range(4):
                                  P.add("pe", lambda e, k=k: e.transpose(pst[:, k * 128:(k + 1) * 128], ynb[:, k * 128:(k + 1) * 128], ident_bf[:]),
                                        ["ynb", "c_ident"], ["pst"])
                              act(ycT[:, 4:8, i * 128:(i + 1) * 128], pst[:, 0:512].rearrange("p (k t) -> p k t", k=4), ACTF.Copy, ["pst"], ["ycTn"])
                      chk("nsa")
                      P.barrier()
                      with ExitStack() as sa4:
                          wo = sbt(sa4, "wo", [128, 8, DM], BF16)
                          load_w(wo[:, :, :], dr["w_out"], 0, DM, "wo")
                          out_proj(ycT, "ycTall", wo, "wo", xres, xru)
                except _Stop:
                    pass
                P.muted = False
                P.barrier()

                if nstage >= 2:
                    with ExitStack() as sbk:
                        xnT = sbt(sbk, "xnTb", [128, 8, S], BF16)
                        qTn = sbt(sbk, "qTn", [128, 8, S], BF16)
                        wA = sbt(sbk, "wA", [128, 8, DM], BF16)
                        wB = sbt(sbk, "wB", [128, 8, DM], BF16)
                        ntm = norm_tmps(sbk)
                        memsb = sbt(sbk, "memsb", [128, 1, DM], F32)
                        mnT = sbt(sbk, "mnT", [128, 8, 256], BF16)
                        kTn = sbt(sbk, "kTn", [128, 8, 256], BF16)
                        vx = sbt(sbk, "vx", [128, 2, 4, 257], BF16)
                        qkt2 = qk_tmps(sbk, 2)
                        pxb = [sbt(sbk, "pxb", [128, 2, 512], BF16) for _ in range(2)]
                        otm = sbt(sbk, "otm", [128, 4, DM], BF16)
                        rdx = sbt(sbk, "rdx", [128, 1], F32)
                        load_w(wA[:, :, :], dr["xk"], 0, DM, "wA")
                        load_w(wB[:, :, :], dr["xv"], 0, DM, "wB")
                        for mt in range(2):
                            P.dma("sp", memsb[:, 0, :], dr["mem"][b, mt * 128:(mt + 1) * 128, :], writes=["memsb"])
                            rms_to_T(ntm, memsb[:, 0, :], ["memsb"], nw["nw_mem"], "c_nw_mem", mnT, slice(mt * 128, (mt + 1) * 128), "mnT", "nB")
                        for i in range(NT):
                            rms_to_T(ntm, xres[:, i, :], [xru(i)], nw["nw_x"], "c_nw_x", xnT, slice(i * 128, (i + 1) * 128), "xnTb", "nB")
                        for h in range(4):
                            pl = []
                            for c in range(2):
                                pb, pu = bank("xb", [0, 1, 2, 3])
                                for k in range(8):
                                    mm(pb[:, 0:256], wA[:, k, (2 * h + c) * 128:(2 * h + c + 1) * 128], mnT[:, k, :], k == 0, k == 7, ["wA", "mnT"], [pu])
                                pl.append((pb, pu))
                            qknorm(qkt2, [pl[0][0][:, 0:256], pl[1][0][:, 0:256]], [pl[0][1], pl[1][1]], 256, ones256, "c_ones256",
                                   [xknw[:, 0:1], xknw[:, 1:2]], "c_xknw", [kTn[:, 2 * h, :], kTn[:, 2 * h + 1, :]], ["kTn", "kTn"], [6], "qx")
                        dve(lambda e: e.memset(vx[:].rearrange("p a b c -> p (a b c)"), 1.0), [], ["vx"])
                        for mt in range(2):
                            for hf in range(2):
                                pb, pu = bank("xb", [0, 1, 2, 3])
                                for k in range(8):
                                    mm(pb[:, :], mnT[:, k, mt * 128:(mt + 1) * 128], wB[:, k, hf * 512:(hf + 1) * 512], k == 0, k == 7, ["wB", "mnT"], [pu])
                                act(vx[:, mt, 2 * hf:2 * hf + 2, 0:256], pb[:, :].rearrange("p (a d) -> p a d", a=2), ACTF.Copy, [pu], ["vx"])
                        load_w(wA[:, :, :], dr["xq"], 0, DM, "wA")
                        load_w(wB[:, :, :], dr["xo"], 0, DM, "wB")
                        for h in range(4):
                            for q in range(4):
                                pl = []
                                for c in range(2):
                                    pb, pu = bank("xb", [0, 1, 2, 3])
                                    for k in range(8):
                                        mm(pb[:, :], wA[:, k, (2 * h + c) * 128:(2 * h + c + 1) * 128], xnT[:, k, q * 512:(q + 1) * 512], k == 0, k == 7, ["wA", "xnTb"], [pu])
                                    pl.append((pb, pu))
                                qknorm(qkt2, [pl[0][0][:, :], pl[1][0][:, :]], [pl[0][1], pl[1][1]], 512, ones256, "c_ones256",
                                       [xqnw[:, 0:1], xqnw[:, 1:2]], "c_xqnw",
                                       [qTn[:, 2 * h, q * 512:(q + 1) * 512], qTn[:, 2 * h + 1, q * 512:(q + 1) * 512]], ["qTn", "qTn"], [6], "qx")
                        pq = [0]
                        pendx = [None]

                        def xa_p1(q, h):
                            px = pxb[pq[0] % 2]
                            pxu = "pxb%d" % (pq[0] % 2)
                            pq[0] += 1
                            for mt in range(2):
                                sb_, su = bank("xs", [0, 1, 2, 3])
                                for c in range(2):
                                    mm(sb_[:, :], kTn[:, 2 * h + c, mt * 128:(mt + 1) * 128], qTn[:, 2 * h + c, q * 512:(q + 1) * 512], c == 0, c == 1, ["kTn", "qTn"], [su])
                                act(px[:, mt, :], sb_[:, :], ACTF.Exp, [su], [pxu])
                            return (h, px, pxu)

                        def xa_p2(d_):
                            h, px, pxu = d_
                            for t in range(4):
                                ob, ou = bank("xo", [4, 5])
                                for mt in range(2):
                                    mm(ob[:, 0:257], px[:, mt, t * 128:(t + 1) * 128], vx[:, mt, h, :], mt == 0, mt == 1, [pxu, "vx"], [ou])
                                dve(lambda e, ob=ob: e.reciprocal(rdx[:], ob[:, 256:257]), [ou], ["rdx"])
                                act(otm[:, t, h * 256:(h + 1) * 256], ob[:, 0:256], ACTF.Copy, [ou, "rdx"], ["otm"], scale=rdx[:])

                        for q in range(4):
                            for h in range(4):
                                d_ = xa_p1(q, h)
                                if pendx[0] is not None:
                                    xa_p2(pendx[0])
                                pendx[0] = d_
                            xa_p2(pendx[0])
                            pendx[0] = None
                            for t in range(4):
                                tm_to_T(otm[:, t, :], "otm", 8, xnT, slice((4 * q + t) * 128, (4 * q + t + 1) * 128), "xnTb")
                        out_proj(xnT, "xnTb", wB, "wB", xres, xru)
                    P.barrier()
                for i in range(NT):
                    P.dma("sp", out[b, i * 128:(i + 1) * 128, :], xres[:, i, :], reads=[xru(i)], writes=["OUT%d_%d" % (b, i)])
            if nstage >= 3:
                P.barrier()
                peer_stage(b)
        P.emit()
    return nc


_CACHE = {}


def kernel(**inputs):
    consts = host_consts()
    prm = host_params(inputs)
    if "nc" not in _CACHE:
        _CACHE["nc"] = build()
    nc = _CACHE["nc"]
    in_maps = []
    for c in range(NCORES):
        m = dict(prm)
        m.update(consts)
        m["x"] = np.ascontiguousarray(inputs["x"][c * NB:(c + 1) * NB], dtype=np.float32)
        m["mem"] = np.ascontiguousarray(inputs["mem"][c * NB:(c + 1) * NB], dtype=np.float32)
        in_maps.append(m)
    res = run_bass_kernel_spmd(nc, in_maps, core_ids=list(range(NCORES)))
    return np.concatenate([r["out"] for r in res.results], axis=0).astype(np.float32)
```
